# Optimizing a Trainium2 kernel written in Bass

```python
import math
import jax, jax.numpy as jnp
from jax import lax
import numpy as np

D_MODEL = 1024
BATCH = 4
SEQ = 4096
DEPTH = 1

HEAD_DIM = 64
ATTN_SCALE = HEAD_DIM ** -0.5
SWA_Q_HEADS = 8
SWA_KV_HEADS = 2
SWA_GROUP = SWA_Q_HEADS // SWA_KV_HEADS
SWA_WINDOW = 128
SWA_BLOCK = 128
MOBA_HEADS = 8
MOBA_BLOCK = 256
MOBA_TOPK = 3
MOBA_Q_CHUNK = 32
NUM_BUCKETS = 32
MAX_EXACT = NUM_BUCKETS // 2
MAX_DISTANCE = 2048
N_ATTN_HEADS = SWA_Q_HEADS + MOBA_HEADS
SWA_Q_W = SWA_Q_HEADS * HEAD_DIM
SWA_KV_W = SWA_KV_HEADS * HEAD_DIM
MOBA_W = MOBA_HEADS * HEAD_DIM
N_BRANCHES = 2
IN_WIDTHS = (SWA_Q_W, SWA_KV_W, SWA_KV_W, MOBA_W, MOBA_W, MOBA_W, D_MODEL, D_MODEL)
IN_TOTAL = SWA_Q_W + 2 * SWA_KV_W + 3 * MOBA_W + N_BRANCHES * D_MODEL
D_FF = 4 * D_MODEL
N_MOD = 6
RMS_EPS = 1e-6

kernel_name = "hybrid_swa_moba_gated_adaln_block"


def rmsnorm(x, g):
    xf = x.astype(jnp.float32)
    y = xf * lax.rsqrt(jnp.mean(xf * xf, axis=-1, keepdims=True) + RMS_EPS)
    return (y * g.astype(jnp.float32)).astype(x.dtype)


def modulate(h, shift, scale):
    return h * (1.0 + scale[:, None, :]) + shift[:, None, :]


def t5_bucket(dist):
    n = jnp.maximum(dist, 0)
    nf = jnp.maximum(n, 1).astype(jnp.float32)
    large = MAX_EXACT + (jnp.log(nf / MAX_EXACT) / math.log(MAX_DISTANCE / MAX_EXACT)
                         * (NUM_BUCKETS - MAX_EXACT)).astype(jnp.int32)
    large = jnp.minimum(large, NUM_BUCKETS - 1)
    return jnp.where(n < MAX_EXACT, n, large)


def swa_attention(q, k, v, sinks, rel_bias):
    B, S = q.shape[0], q.shape[1]
    L = SWA_BLOCK
    nb = S // L
    qb = q.reshape(B, nb, L, SWA_KV_HEADS, SWA_GROUP, HEAD_DIM)
    kb = k.reshape(B, nb, L, SWA_KV_HEADS, HEAD_DIM)
    vb = v.reshape(B, nb, L, SWA_KV_HEADS, HEAD_DIM)
    pad = ((0, 0), (1, 0), (0, 0), (0, 0), (0, 0))
    kk = jnp.concatenate([jnp.pad(kb, pad)[:, :-1], kb], axis=2)
    vv = jnp.concatenate([jnp.pad(vb, pad)[:, :-1], vb], axis=2)
    s = jnp.einsum('bnqhgd,bnkhd->bnhgqk', qb, kk).astype(jnp.float32) * ATTN_SCALE
    i = jnp.arange(L)[:, None] + L
    j = jnp.arange(2 * L)[None, :]
    dist = i - j
    bias = rel_bias[t5_bucket(dist)][..., :SWA_Q_HEADS].astype(jnp.float32)
    bias = bias.transpose(2, 0, 1).reshape(SWA_KV_HEADS, SWA_GROUP, L, 2 * L)
    kabs = jnp.arange(nb)[:, None, None] * L - L + j[None]
    mask = ((dist >= 0) & (dist < SWA_WINDOW))[None] & (kabs >= 0)
    s = jnp.where(mask[None, :, None, None], s + bias, -jnp.inf)
    sink = sinks.astype(jnp.float32).reshape(SWA_KV_HEADS, SWA_GROUP)[None, None, :, :, None, None]
    m = jnp.maximum(jnp.max(s, axis=-1, keepdims=True), sink)
    p = jnp.exp(s - m)
    p = p / (jnp.sum(p, axis=-1, keepdims=True) + jnp.exp(sink - m))
    o = jnp.einsum('bnhgqk,bnkhd->bnqhgd', p.astype(v.dtype), vv)
    return o.reshape(B, S, SWA_Q_HEADS * HEAD_DIM)


def moba_attention(q, k, v, rel_bias):
    B, S = q.shape[0], q.shape[1]
    H = MOBA_HEADS
    MB = MOBA_BLOCK
    nblk = (S + MB - 1) // MB
    Sp = nblk * MB
    k_eff = max(1, min(MOBA_TOPK, nblk - 1))
    padw = ((0, 0), (0, Sp - S), (0, 0), (0, 0))
    q = jnp.pad(q, padw)
    k = jnp.pad(k, padw)
    v = jnp.pad(v, padw)
    qh = q.transpose(0, 2, 1, 3)
    kb = k.reshape(B, nblk, MB, H, HEAD_DIM).transpose(0, 3, 1, 2, 4)
    vb = v.reshape(B, nblk, MB, H, HEAD_DIM).transpose(0, 3, 1, 2, 4)
    kmean = jnp.mean(kb.astype(jnp.float32), axis=3)
    gate = jnp.einsum('bhsd,bhnd->bhsn', qh.astype(jnp.float32), kmean)
    qblk = jnp.arange(Sp) // MB
    past = jnp.arange(nblk)[None, :] < qblk[:, None]
    gate = jnp.where(past, gate, -jnp.inf)
    _, idx = lax.top_k(gate, k_eff)
    valid = jnp.arange(k_eff)[None, :] < qblk[:, None]
    table_b = rel_bias[:, SWA_Q_HEADS:].T.astype(jnp.float32)
    bi = jnp.arange(B)[:, None, None, None]
    hi = jnp.arange(H)[None, :, None, None]
    hi5 = hi[..., None]
    n_chunks = Sp // MOBA_Q_CHUNK

    def chunk_fn(ci):
        start = ci * MOBA_Q_CHUNK
        q_c = lax.dynamic_slice_in_dim(qh, start, MOBA_Q_CHUNK, axis=2)
        idx_c = lax.dynamic_slice_in_dim(idx, start, MOBA_Q_CHUNK, axis=2)
        valid_c = lax.dynamic_slice_in_dim(valid, start, MOBA_Q_CHUNK, axis=0)
        qpos = start + jnp.arange(MOBA_Q_CHUNK)
        own = start // MB
        k_sel = kb[bi, hi, idx_c]
        v_sel = vb[bi, hi, idx_c]
        s_past = jnp.einsum('bhqd,bhqrkd->bhqrk', q_c, k_sel).astype(jnp.float32) * ATTN_SCALE
        kpos_past = idx_c[..., None] * MB + jnp.arange(MB)
        bias_past = table_b[hi5, t5_bucket(qpos[:, None, None] - kpos_past)]
        s_past = jnp.where(valid_c[None, None, :, :, None], s_past + bias_past, -jnp.inf)
        s_past = s_past.reshape(B, H, MOBA_Q_CHUNK, k_eff * MB)
        k_own = lax.dynamic_index_in_dim(kb, own, axis=2, keepdims=False)
        v_own = lax.dynamic_index_in_dim(vb, own, axis=2, keepdims=False)
        s_own = jnp.einsum('bhqd,bhkd->bhqk', q_c, k_own).astype(jnp.float32) * ATTN_SCALE
        dist_own = qpos[:, None] - (own * MB + jnp.arange(MB))[None, :]
        bias_own = table_b[:, t5_bucket(dist_own)]
        s_own = jnp.where(dist_own >= 0, s_own + bias_own, -jnp.inf)
        p = jax.nn.softmax(jnp.concatenate([s_past, s_own], axis=-1), axis=-1)
        p_past = p[..., :k_eff * MB].reshape(B, H, MOBA_Q_CHUNK, k_eff, MB)
        p_own = p[..., k_eff * MB:]
        o = (jnp.einsum('bhqrk,bhqrkd->bhqd', p_past.astype(v_sel.dtype), v_sel)
             + jnp.einsum('bhqk,bhkd->bhqd', p_own.astype(v_own.dtype), v_own))
        return o

    out = lax.map(chunk_fn, jnp.arange(n_chunks))
    out = out.transpose(1, 0, 3, 2, 4).reshape(B, Sp, H * HEAD_DIM)
    return out[:, :S]


def setup_inputs(seed: int = 0) -> dict:
    key = jax.random.key(seed)
    ks = jax.random.split(key, 16)
    f32 = jnp.float32
    nrm = lambda k, shape, s: jax.random.normal(k, shape, f32) * s
    return {
        "x": nrm(ks[0], (BATCH, SEQ, D_MODEL), 1.0),
        "c": nrm(ks[1], (BATCH, D_MODEL), 1.0),
        "ada_w": nrm(ks[2], (DEPTH, D_MODEL, N_MOD * D_MODEL), D_MODEL ** -0.5),
        "ada_b": nrm(ks[3], (DEPTH, N_MOD * D_MODEL), 0.02),
        "norm1_g": 1.0 + nrm(ks[4], (DEPTH, D_MODEL), 0.02),
        "norm2_g": 1.0 + nrm(ks[5], (DEPTH, D_MODEL), 0.02),
        "w_in": nrm(ks[6], (DEPTH, D_MODEL, IN_TOTAL), D_MODEL ** -0.5),
        "attn_sinks": nrm(ks[7], (DEPTH, SWA_Q_HEADS), 0.5),
        "rel_bias": nrm(ks[8], (NUM_BUCKETS, N_ATTN_HEADS), 0.5),
        "w_branch_a": nrm(ks[9], (DEPTH, SWA_Q_W, D_MODEL), SWA_Q_W ** -0.5),
        "w_branch_b": nrm(ks[10], (DEPTH, MOBA_W, D_MODEL), MOBA_W ** -0.5),
        "w_out": nrm(ks[11], (DEPTH, D_MODEL, D_MODEL), D_MODEL ** -0.5),
        "w_mlp_in": nrm(ks[12], (DEPTH, D_MODEL, D_FF), D_MODEL ** -0.5),
        "w_mlp_out": nrm(ks[13], (DEPTH, D_FF, D_MODEL), D_FF ** -0.5),
        "final_g": 1.0 + nrm(ks[14], (D_MODEL,), 0.02),
    }


def reference(x, c, ada_w, ada_b, norm1_g, norm2_g, w_in, attn_sinks, rel_bias,
              w_branch_a, w_branch_b, w_out, w_mlp_in, w_mlp_out, final_g):
    B, S = x.shape[0], x.shape[1]
    offsets = []
    acc = 0
    for w in IN_WIDTHS[:-1]:
        acc += w
        offsets.append(acc)
    cs = jax.nn.silu(c)
    for l in range(DEPTH):
        mod = cs @ ada_w[l] + ada_b[l]
        shift1, scale1, gate1, shift2, scale2, gate2 = jnp.split(mod, N_MOD, axis=-1)
        h = modulate(rmsnorm(x, norm1_g[l]), shift1, scale1)
        proj = h @ w_in[l]
        qa, ka, va, qb, kb, vb, ga, gb = jnp.split(proj, offsets, axis=-1)
        ya = swa_attention(qa.reshape(B, S, SWA_Q_HEADS, HEAD_DIM),
                           ka.reshape(B, S, SWA_KV_HEADS, HEAD_DIM),
                           va.reshape(B, S, SWA_KV_HEADS, HEAD_DIM),
                           attn_sinks[l], rel_bias) @ w_branch_a[l]
        yb = moba_attention(qb.reshape(B, S, MOBA_HEADS, HEAD_DIM),
                            kb.reshape(B, S, MOBA_HEADS, HEAD_DIM),
                            vb.reshape(B, S, MOBA_HEADS, HEAD_DIM),
                            rel_bias) @ w_branch_b[l]
        merged = jax.nn.sigmoid(ga) * ya + jax.nn.sigmoid(gb) * yb
        x = x + gate1[:, None, :] * (merged @ w_out[l])
        h2 = modulate(rmsnorm(x, norm2_g[l]), shift2, scale2)
        y = jnp.square(jax.nn.relu(h2 @ w_mlp_in[l])) @ w_mlp_out[l]
        x = x + gate2[:, None, :] * y
    return rmsnorm(x, final_g)
```

```python
import numpy as np
import ml_dtypes
import concourse.bass as bass
import concourse.mybir as mybir
from concourse.bass_utils import run_bass_kernel_spmd

F32 = mybir.dt.float32
BF16 = mybir.dt.bfloat16
AF = mybir.ActivationFunctionType
ALU = mybir.AluOpType
AX = mybir.AxisListType

NEG = -30000.0
SB_LIMIT = 212800
SB_BASE = 16512
D = 1024
NT_OWN = 16
LT_M = 3072
LW_M = 2560
LT_A = 512
LW_A = 384
DEBUG = None
PEB_MOD = 100000


def _isz(dt):
    return 4 if dt == F32 else 2


class Buf:
    def __init__(self, h, space, base, size, shape, dtype):
        self.h, self.space, self.base, self.size, self.shape, self.dtype = h, space, base, size, shape, dtype
        inner = 1
        for s in shape[2:]:
            inner *= s
        self.inner = inner

    def ap(self):
        return self.h.ap()

    def sl(self, i, n=1):
        e = _isz(self.dtype)
        return (self, i * self.inner * e, (i + n) * self.inner * e)

    def el(self, lo, hi):
        e = _isz(self.dtype)
        return (self, lo * e, hi * e)


def _norm_region(r):
    if isinstance(r, Buf):
        return (r.space, r.base, r.base + r.size)
    b, lo, hi = r
    return (b.space, b.base + lo, b.base + hi)


class Sched:
    def __init__(self, nc):
        self.nc = nc
        self.ops = []

    def add(self, eng, fn, reads=(), writes=(), key=None):
        rr = [_norm_region(r) for r in reads]
        ww = [_norm_region(w) for w in writes]
        if key is not None:
            ww.append(('key:' + key, 0, 1))
        self.ops.append((eng, fn, rr, ww, key))

    def emit(self):
        nc = self.nc
        ops = self.ops
        n = len(ops)
        handles = {'pe': nc.tensor, 'dve': nc.vector, 'act': nc.scalar, 'pool': nc.gpsimd, 'sp': nc.sync}
        writers = {}
        readers = {}
        deps = [None] * n
        needs = [False] * n
        for i, (eng, fn, rds, wrs, key) in enumerate(ops):
            d = set()
            for (sp, lo, hi) in rds:
                for w in writers.get(sp, ()):
                    if w[0] < hi and lo < w[1]:
                        d.add(w[2])
            for (sp, lo, hi) in wrs:
                for w in writers.get(sp, ()):
                    if w[0] < hi and lo < w[1]:
                        d.add(w[2])
                for r in readers.get(sp, ()):
                    if r[0] < hi and lo < r[1]:
                        d.add(r[2])
            d.discard(i)
            if eng == 'pe':
                d = {j for j in d if ops[j][0] != 'pe' or ops[j][4] is not None}
            deps[i] = d
            for j in d:
                needs[j] = True
            ek = eng if key is None else ('dma', key)
            for (sp, lo, hi) in rds:
                lst = readers.setdefault(sp, [])
                lst[:] = [r for r in lst if not (r[3] == ek and r[0] >= lo and r[1] <= hi)]
                lst.append([lo, hi, i, ek])
            for (sp, lo, hi) in wrs:
                lst = readers.setdefault(sp, [])
                lst[:] = [r for r in lst if not (r[0] >= lo and r[1] <= hi)]
                wl = writers.setdefault(sp, [])
                wl[:] = [w for w in wl if not (w[0] >= lo and w[1] <= hi)]
                wl.append([lo, hi, i])
        sem_eng = {e: nc.alloc_semaphore(f"se_{e}") for e in ('pe', 'dve', 'act', 'pool')}
        cnt_eng = {e: 0 for e in sem_eng}
        dma_sems = {}
        done = [None] * n
        seen = {}
        nwait = 0
        for i, (eng, fn, rds, wrs, key) in enumerate(ops):
            h = handles[eng]
            for dj in sorted(deps[i]):
                if done[dj] is None:
                    raise RuntimeError(f"dep {dj} of op {i} has no completion")
                sname, semobj, val = done[dj]
                if seen.get((eng, sname), 0) >= val:
                    continue
                h.wait_ge(semobj, val)
                nwait += 1
                seen[(eng, sname)] = val
            res = fn(h)
            if key is not None:
                insts = res if isinstance(res, list) else [res]
                if key not in dma_sems:
                    dma_sems[key] = [nc.alloc_semaphore(f"sd_{len(dma_sems)}"), 0]
                ent = dma_sems[key]
                for ins in insts:
                    ins.then_inc(ent[0], 16)
                    ent[1] += 16
                done[i] = (('dma', key), ent[0], ent[1])
            elif eng == 'sp':
                done[i] = None
            else:
                if needs[i]:
                    cnt_eng[eng] += 1
                    res.then_inc(sem_eng[eng], 1)
                    done[i] = (eng, sem_eng[eng], cnt_eng[eng])
                else:
                    done[i] = (eng, sem_eng[eng], cnt_eng[eng] + 0) if False else None
        return dict(nops=n, nwait=nwait, cnt=cnt_eng, ndma=len(dma_sems))


def t5_bucket_np(d):
    d = np.asarray(d)
    n = np.maximum(d, 0)
    nf = np.maximum(n, 1).astype(np.float32)
    large = 16 + (np.log(nf / np.float32(16)) / np.float32(np.log(2048 / 16)) * np.float32(16)).astype(np.int32)
    large = np.minimum(large, 31)
    return np.where(n < 16, n, large)


def build_program(debug=None):
    nc = bass.Bass("TRN2", target_bir_lowering=False)
    S = Sched(nc)
    cnt = [0]

    def dram(name, shape, dt, kind):
        h = nc.dram_tensor(name, shape, dt, kind=kind)
        sz = 1
        for s in shape:
            sz *= s
        return Buf(h, 'dram:' + name, 0, sz * _isz(dt), [1] + list(shape), dt)

    def sb(name, shape, dt, off):
        assert off % 32 == 0, (name, off)
        cnt[0] += 1
        sz = _isz(dt)
        for s in shape[1:]:
            sz *= s
        assert off + sz <= SB_LIMIT, (name, off, sz)
        h = nc.alloc_sbuf_tensor_at(f"{name}_{cnt[0]}", list(shape), dt, offset=SB_BASE + off)
        return Buf(h, 'sb', off, sz, list(shape), dt)

    I = 'ExternalInput'
    x_own = dram("x_own", [2048, D], F32, I)
    x_oth = dram("x_oth", [2048, D], F32, I)
    ccol = dram("ccol", [128, 8], F32, I)
    flag_d = dram("flag", [128, 1], F32, I)
    ada_w = dram("ada_w", [D, 6 * D], F32, I)
    ada_b = dram("ada_b", [1, 6 * D], F32, I)
    g1_d = dram("g1", [1, D], F32, I)
    g2_d = dram("g2", [1, D], F32, I)
    gf_d = dram("gf", [1, D], F32, I)
    wqkv_d = dram("wqkv", [D, 2304], F32, I)
    wg_d = dram("wg", [D, 2048], F32, I)
    sinks_d = dram("sinks", [1, 8], F32, I)
    relb_d = dram("relb", [32, 16], F32, I)
    wba_d = dram("wba", [512, D], F32, I)
    wbb_d = dram("wbb", [512, D], F32, I)
    wout_d = dram("wout", [D, D], F32, I)
    wmi_d = dram("wmi", [D, 4 * D], F32, I)
    wmo_d = dram("wmo", [4 * D, D], F32, I)
    ident_d = dram("ident", [128, 128], BF16, I)
    ohm_d = dram("ohm", [33, LT_M], BF16, I)
    oha_d = dram("oha", [33, LT_A], BF16, I)
    blkoh_d = dram("blkoh", [16, 4096], BF16, I)
    out_d = dram("out", [2048, D], F32, 'ExternalOutput')
    mod_d = dram("mod_scr", [1, 6 * D], F32, 'Internal')
    scrm_d = dram("scr_m", [8, 128, LT_M], BF16, 'Internal')
    scra_d = dram("scr_a", [8, 128, LT_A], BF16, 'Internal')
    dbg_d = None
    if debug:
        dbg_d = dram("dbg", [128, 8192], F32, 'ExternalOutput')

    PS = []
    for b in range(8):
        h = nc.alloc_psum_tensor(f"psb{b}", [128, 512], F32)
        PS.append(Buf(h, 'ps', b * 2048, 2048, [128, 512], F32))

    def ps_bf(b):
        return PS[b].ap().bitcast(BF16)

    def dma(eng, key, out_ap, in_ap, reads, writes):
        S.add(eng, lambda h, o=out_ap, i=in_ap: h.dma_start(out=o, in_=i), reads, writes, key=key)

    def bcast_rows(d_ap_row, nparts=128):
        t = d_ap_row
        return bass.AP(t.tensor, t.offset, [[0, nparts]] + [list(x) for x in t.ap[1:]])

    ident = sb("ident", [128, 128], BF16, 0)
    onesf = sb("onesf", [128, 128], F32, 256)
    flag = sb("flag", [128, 4], F32, 768)
    esink = sb("esink", [128, 8], F32, 800)
    rb31 = sb("rb31", [128, 16], F32, 832)
    cs = sb("cs", [128, 8], F32, 896)
    epsb = sb("epsb", [128, 1], F32, 928)
    ccs = sb("ccs", [128, 8], F32, 960)
    pm = sb("pm", [128, 16, 16], F32, 1024)
    basem = sb("basem", [128, 16, 16], F32, 2048)
    zeros = sb("zeros", [128, 128], F32, 3072)
    stat = sb("stat", [128, 8], F32, 3584)

    dma('sp', 'c_ident', ident.ap(), ident_d.ap(), [ident_d], [ident])
    dma('sp', 'c_flag', flag.ap()[:, 0:1], flag_d.ap(), [flag_d], [flag])
    dma('sp', 'c_cc', ccs.ap(), ccol.ap(), [ccol], [ccs])
    dma('sp', 'c_sink', esink.ap(), bcast_rows(sinks_d.ap()), [sinks_d], [esink])
    dma('sp', 'c_rb31', rb31.ap(), bcast_rows(relb_d.ap()[31:32, :]), [relb_d], [rb31])
    S.add('dve', lambda h: h.memset(onesf.ap(), 1.0), [], [onesf])
    S.add('dve', lambda h: h.memset(zeros.ap(), 0.0), [], [zeros])
    S.add('dve', lambda h: h.memset(epsb.ap(), 1e-6), [], [epsb])
    S.add('dve', lambda h: h.tensor_scalar(out=flag.ap()[:, 1:2], in0=flag.ap()[:, 0:1], scalar1=NEG, scalar2=None,
                                           op0=ALU.mult), [flag], [flag])
    S.add('dve', lambda h: h.tensor_scalar(out=flag.ap()[:, 2:3], in0=flag.ap()[:, 0:1], scalar1=-2e30, scalar2=None,
                                           op0=ALU.mult), [flag], [flag])
    S.add('act', lambda h: h.activation(out=esink.ap(), in_=esink.ap(), func=AF.Exp), [esink], [esink])
    S.add('act', lambda h: h.activation(out=cs.ap(), in_=ccs.ap(), func=AF.Silu), [ccs], [cs])
    S.add('pool', lambda h: h.memset(pm.ap(), -2e30), [], [pm])
    S.add('pool', lambda h: h.memset(basem.ap(), NEG), [], [basem])
    for s in range(8):
        S.add('pool', lambda h, s=s: h.memset(pm.ap()[:, 2 * s:2 * s + 2, 0:2 * s + 1], 0.0), [], [pm])
        S.add('pool', lambda h, s=s: h.memset(basem.ap()[:, 2 * s:2 * s + 2, 2 * s + 1:2 * s + 2], 0.0), [], [basem])
    S.add('dve', lambda h: h.tensor_scalar(out=pm.ap()[:, :, 0:1], in0=pm.ap()[:, :, 0:1], scalar1=flag.ap()[:, 2:3],
                                           scalar2=None, op0=ALU.add), [pm, flag], [pm])

    O_P0 = 4096
    rbs = sb("rbs", [33, 16], F32, O_P0)
    rbB = sb("rbB", [33, 16, 128], BF16, O_P0 + 64)
    ohm = sb("ohm", [33, LT_M], BF16, O_P0 + 4160)
    oha = sb("oha", [33, LT_A], BF16, O_P0 + 10304)
    tst = [sb("tst", [128, LT_M], BF16, O_P0 + 11328 + i * 6144) for i in range(2)]
    mp1 = [sb("mp1", [128, 2048], F32, o_) for o_ in (70144, 78336, 103040, 111232)]

    def mod_pass(c0, ncols, bufs, acc, keyp, bank_fn, eng='dve', dq='sp', rowbuf=None):
        nb = len(bufs)

        def load(k):
            if k < 8:
                dma(dq, f'{keyp}{k % nb}', bufs[k % nb].ap()[:, 0:ncols], ada_w.ap()[k * 128:(k + 1) * 128, c0:c0 + ncols],
                    [ada_w], [bufs[k % nb]])
            else:
                dma(dq, f'{keyp}{k % nb}', bufs[k % nb].ap()[0:1, 0:ncols], ada_b.ap()[:, c0:c0 + ncols], [ada_b], [bufs[k % nb]])

        def accf(k):
            bw = bufs[k % nb]
            if k == 0:
                S.add(eng, lambda h: h.tensor_scalar(out=acc.ap()[:, 0:ncols], in0=bw.ap()[:, 0:ncols], scalar1=cs.ap()[:, 0:1], scalar2=0.0,
                                                     op0=ALU.mult, op1=ALU.add), [bw, cs], [acc])
            elif k < 8 and eng == 'dve':
                S.add('dve', lambda h: h.scalar_tensor_tensor(out=acc.ap()[:, 0:ncols], in0=bw.ap()[:, 0:ncols], scalar=cs.ap()[:, k:k + 1],
                                                              in1=acc.ap()[:, 0:ncols], op0=ALU.mult, op1=ALU.add), [bw, cs, acc], [acc])
            elif k < 8:
                S.add(eng, lambda h: h.tensor_scalar(out=bw.ap()[:, 0:ncols], in0=bw.ap()[:, 0:ncols], scalar1=cs.ap()[:, k:k + 1], scalar2=0.0,
                                                     op0=ALU.mult, op1=ALU.add), [bw, cs], [bw])
                S.add(eng, lambda h: h.tensor_tensor(out=acc.ap()[:, 0:ncols], in0=acc.ap()[:, 0:ncols], in1=bw.ap()[:, 0:ncols], op=ALU.add),
                      [bw, acc], [acc])
            else:
                S.add(eng, lambda h: h.tensor_tensor(out=acc.ap()[0:1, 0:ncols], in0=acc.ap()[0:1, 0:ncols], in1=bw.ap()[0:1, 0:ncols], op=ALU.add),
                      [bw, acc], [acc])

        def fin():
            ob_ = acc if rowbuf is None else rowbuf
            for p0 in range(0, ncols, 512):
                pb = bank_fn()
                S.add('pe', lambda h, p0=p0, pb=pb: h.matmul(pb.ap()[0:1, :], lhsT=onesf.ap()[:, 0:1], rhs=acc.ap()[:, p0:p0 + 512], start=True, stop=True),
                      [onesf, acc], [pb])
                S.add('dve', lambda h, p0=p0, pb=pb: h.tensor_copy(out=ob_.ap()[0:1, p0:p0 + 512], in_=pb.ap()[0:1, :]), [pb],
                      [ob_.el(p0, p0 + 512) if rowbuf is not None else ob_])
            dma('sp', f'{keyp}st', mod_d.ap()[:, c0:c0 + ncols], ob_.ap()[0:1, 0:ncols], [ob_], [mod_d.el(c0, c0 + ncols)])
        return load, accf, fin

    p0ctr = [0]

    def p0_bank():
        p0ctr[0] += 1
        return PS[p0ctr[0] % 4]

    deferred_fin = []
    modrow = sb("modrow", [1, 2048], F32, 27712)
    ld_, ac_, fin_ = mod_pass(0, 2048, mp1[0:3], mp1[3], 'mp0', p0_bank, eng='dve', rowbuf=modrow)
    for k in range(3):
        ld_(k)
    for k in range(9):
        ac_(k)
        if k + 3 < 9:
            ld_(k + 3)
    fin_()

    def emit_t5():
        dma('sp', 'c_rbs', rbs.ap()[0:32, :], relb_d.ap(), [relb_d], [rbs])
        dma('sp', 'c_ohm', ohm.ap(), ohm_d.ap(), [ohm_d], [ohm])
        dma('sp', 'c_oha', oha.ap(), oha_d.ap(), [oha_d], [oha])
        S.add('dve', lambda h: h.memset(rbB.ap(), NEG), [], [rbB])
        S.add('dve', lambda h: h.tensor_copy(out=rbB.ap()[0:32, :, :],
                                             in_=rbs.ap()[0:32, :].unsqueeze(2).to_broadcast([32, 16, 128])), [rbs, rbB], [rbB])
        tcount = 0
        for hh in range(16):
            is_a = hh < 8
            oh = oha if is_a else ohm
            LT = LT_A if is_a else LT_M
            st = tst[hh % 2]
            nch = LT // 512
            for ch in range(nch):
                pb = PS[2 + (tcount % 2)]
                tcount += 1
                S.add('pe', lambda h, pb=pb, hh=hh, oh=oh, ch=ch: h.matmul(
                    pb.ap(), lhsT=rbB.ap()[0:33, hh, :], rhs=oh.ap()[0:33, ch * 512:(ch + 1) * 512], start=True, stop=True),
                    [rbB, oh], [pb])
                S.add('act', lambda h, pb=pb, st=st, ch=ch: h.copy(out=st.ap()[:, ch * 512:(ch + 1) * 512], in_=pb.ap()),
                      [pb], [st.el(ch * 512, (ch + 1) * 512)])
            if is_a:
                dma('sp', f'tst{hh % 2}', scra_d.ap()[hh], st.ap()[:, 0:LT_A], [st],
                    [scra_d.el(hh * 128 * LT_A, (hh + 1) * 128 * LT_A)])
            else:
                dma('sp', f'tst{hh % 2}', scrm_d.ap()[hh - 8], st.ap(), [st],
                    [scrm_d.el((hh - 8) * 128 * LT_M, (hh - 7) * 128 * LT_M)])


    O_K = 4096
    Kst = sb("Kst", [128, 4, 4096], BF16, O_K)
    O_V = O_K + 32768
    Vb = sb("Vb", [128, 32, 8, 65], BF16, O_V)
    O_Q = O_V + 33280
    Qst = sb("Qst", [128, 4, 2048], BF16, O_Q)
    O_SWA = O_Q + 16384
    kaT = sb("kaT", [128, 4096], BF16, O_SWA)
    va = sb("va", [128, 32, 2, 65], BF16, O_SWA + 8192)
    qaT = sb("qaT", [128, 4, 2048], BF16, O_SWA + 16512)
    O_W1 = O_SWA + 32896
    wkv = sb("wkv", [128, 8, 1280], BF16, O_W1)
    wq = sb("wq", [128, 8, 1024], BF16, O_W1 + 20480)
    O_F = O_W1 + 36864
    G1bc = sb("G1bc", [128, D], F32, O_F)
    SH1bc = sb("SH1bc", [128, D], F32, O_F + 4096)
    xt = [sb("xt", [128, D], F32, O_F + 8192 + i * 4096) for i in range(2)]
    hb = [sb("hb", [128, D], BF16, O_F + 16384 + i * 2048) for i in range(2)]
    hT = [sb("hT", [128, 8, 512], BF16, O_F + 20480 + i * 8192) for i in range(2)]
    g1bc = sb("g1bc", [128, D], F32, O_F + 36896)
    xt.append(sb("xt", [128, D], F32, O_F + 36896 + 4096))
    hb.append(sb("hb", [128, D], BF16, O_F + 36896))

    def load_mod_bc(dst, idx, key):
        dma('sp', key, dst.ap(), bcast_rows(mod_d.ap()[:, idx * D:(idx + 1) * D]), [mod_d.el(idx * D, (idx + 1) * D)], [dst])

    dma('sp', 'c_g1b', g1bc.ap(), bcast_rows(g1_d.ap()), [g1_d], [g1bc])
    for pc in range(4):
        bb_ = PS[4 + pc]
        S.add('pe', lambda h, pc=pc, bb_=bb_: h.matmul(bb_.ap(), lhsT=onesf.ap()[0:1, 0:128], rhs=modrow.ap()[0:1, pc * 512:(pc + 1) * 512],
                                                       start=True, stop=True), [onesf, modrow.el(pc * 512, (pc + 1) * 512)], [bb_])
        if pc < 2:
            S.add('act', lambda h, pc=pc, bb_=bb_: h.copy(out=SH1bc.ap()[:, pc * 512:(pc + 1) * 512], in_=bb_.ap()), [bb_],
                  [SH1bc.el(pc * 512, (pc + 1) * 512)])
        else:
            c0_ = (pc - 2) * 512
            S.add('dve', lambda h, c0_=c0_, bb_=bb_: h.scalar_tensor_tensor(out=G1bc.ap()[:, c0_:c0_ + 512], in0=bb_.ap(), scalar=1.0,
                                                                            in1=g1bc.ap()[:, c0_:c0_ + 512], op0=ALU.add, op1=ALU.mult),
                  [bb_, g1bc], [G1bc.el(c0_, c0_ + 512)])

    def load_w(dst, d_view_pkn, ncols, key, src_buf, col0=0, kchunks=8, dst_k0=0):
        step = 1024 if ncols % 1024 == 0 else (1152 if ncols % 1152 == 0 else ncols)
        for k in range(kchunks):
            def f(h, k=k):
                res = []
                for c0 in range(0, ncols, step):
                    res.append(h.dma_start(out=dst.ap()[:, dst_k0 + k, col0 + c0:col0 + c0 + step],
                                           in_=d_view_pkn[:, k, c0:c0 + step]))
                return res
            S.add('pool', f, [src_buf], [dst.sl(dst_k0 + k)], key=f'{key}_{k % 4}')


    wqkv_v = wqkv_d.ap().rearrange("(k p) n -> p k n", p=128)
    load_w(wkv, wqkv_v[:, :, 1024:2304], 1280, 'w_kv', wqkv_d)
    load_w(wq, wqkv_v[:, :, 0:1024], 1024, 'w_kv', wqkv_d)

    S.add('pool', lambda h: h.memset(Vb.ap()[:, :, :, 64:65], 1.0), [], [Vb])
    S.add('pool', lambda h: h.memset(va.ap()[:, :, :, 64:65], 1.0), [], [va])

    stat2 = [stat, sb("statb", [128, 8], F32, 3616)]
    fe_ctr = [0]

    def frontend(src_ap, src_reads, xbuf, hbuf, hTbuf, tcol, Gbc, SHbc, psb, xkey=None):
        st_ = stat2[fe_ctr[0] % 2]
        fe_ctr[0] += 1

        def elem():
            dma('sp', xkey, xbuf.ap(), src_ap, src_reads, [xbuf])
            S.add('act', lambda h: h.activation(out=hbuf.ap(), in_=xbuf.ap(), func=AF.Square, accum_out=st_.ap()[:, 0:1]),
                  [xbuf], [hbuf, st_])
            S.add('act', lambda h: h.activation(out=st_.ap()[:, 1:2], in_=st_.ap()[:, 0:1], func=AF.Sqrt,
                                                bias=epsb.ap()[:, 0:1], scale=1.0 / D), [st_, epsb], [st_])
            S.add('dve', lambda h: h.reciprocal(out=st_.ap()[:, 2:3], in_=st_.ap()[:, 1:2]), [st_], [st_])
            S.add('dve', lambda h: h.scalar_tensor_tensor(out=xbuf.ap(), in0=xbuf.ap(), scalar=st_.ap()[:, 2:3], in1=Gbc.ap(),
                                                          op0=ALU.mult, op1=ALU.mult), [xbuf, st_, Gbc], [xbuf])
            S.add('dve', lambda h: h.tensor_tensor(out=hbuf.ap(), in0=xbuf.ap(), in1=SHbc.ap(), op=ALU.add),
                  [xbuf, SHbc], [hbuf])

        def tr_():
            pv = ps_bf(psb).rearrange("p (c t) -> p c t", c=8)

            def tr(h):
                r = None
                for c in range(8):
                    r = h.transpose(out=pv[:, c, :], in_=hbuf.ap()[:, c * 128:(c + 1) * 128], identity=ident.ap())
                return r
            S.add('pe', tr, [hbuf, ident], [PS[psb]])
            S.add('act', lambda h: h.copy(out=hTbuf.ap()[:, :, tcol * 128:(tcol + 1) * 128], in_=pv), [PS[psb]], [hTbuf])
        return elem, tr_

    def interleave(units, extras):
        n = len(units)
        sched = {}
        for pos, f in extras:
            sched.setdefault(max(0, min(n - 1, pos)), []).append(f)
        for u in range(n):
            for f in sched.get(u, []):
                f()
            units[u]()

    evac_rr = [0]

    def evac(out_ap, in_ap, reads, writes, scale=None):
        e = 'act' if evac_rr[0] % 2 == 0 else 'dve'
        evac_rr[0] += 1
        if e == 'act':
            if scale is None:
                S.add('act', lambda h: h.copy(out=out_ap, in_=in_ap), reads, writes)
            else:
                S.add('act', lambda h: h.activation(out=out_ap, in_=in_ap, func=AF.Identity, scale=scale), reads, writes)
        else:
            if scale is None:
                S.add('dve', lambda h: h.tensor_copy(out=out_ap, in_=in_ap), reads, writes)
            else:
                S.add('dve', lambda h: h.tensor_scalar(out=out_ap, in0=in_ap, scalar1=scale, scalar2=None, op0=ALU.mult),
                      reads, writes)

    proj_rr = [0]

    def next_bank():
        b = 2 + (proj_rr[0] % 5)
        proj_rr[0] += 1
        return b

    def fm_proj(hTbuf, wbuf, wcol0, nk, out_ap, writes, scale=None, rhs_fn=None):
        b = next_bank()

        def mm(h):
            r = None
            for k in range(nk):
                rhs = hTbuf.ap()[:, k, :] if rhs_fn is None else rhs_fn(k)
                r = h.matmul(PS[b].ap(), lhsT=wbuf.ap()[:, k, wcol0:wcol0 + 128], rhs=rhs, start=(k == 0), stop=(k == nk - 1))
            return r
        S.add('pe', mm, [hTbuf, wbuf], [PS[b]])
        evac(out_ap, PS[b].ap(), [PS[b]], writes, scale)

    def phase1_frontend(g, pos):
        own = g < 4
        src = x_own if own else x_oth
        gg = g if own else g - 4
        res = []
        for t in range(4):
            tile_i = gg * 4 + t
            idx = g * 4 + t
            res.append(frontend(src.ap()[tile_i * 128:(tile_i + 1) * 128, :], [src], xt[idx % 3], hb[idx % 3], hT[pos % 2], t,
                                G1bc, SH1bc, idx % 2, xkey=f'xt{idx % 3}'))
        return res

    def phase1_proj(g, pos):
        units = []
        own = g < 4
        gg = g if own else g - 4
        hTb = hT[pos % 2]
        vb0 = (4 * gg + 1) if own else (4 * gg)
        kcol = vb0 * 256
        for c in range(4):
            dst = Kst.ap()[:, c, :]
            dst = bass.AP(dst.tensor, dst.offset + kcol, [list(dst.ap[0]), [512, 2], [1, 256]])
            units.append(lambda c=c, dst=dst: fm_proj(hTb, wkv, c * 128, 8, dst, [Kst.sl(c)]))
        dst = kaT.ap()
        dst = bass.AP(dst.tensor, dst.offset + kcol, [list(dst.ap[0]), [512, 2], [1, 256]])
        units.append(lambda dst=dst: fm_proj(hTb, wkv, 512, 8, dst, [kaT]))
        if own:
            for c in range(4):
                units.append(lambda c=c: fm_proj(hTb, wq, c * 128, 8, qaT.ap()[:, c, gg * 512:(gg + 1) * 512], [qaT.sl(c)], scale=0.125))
            for c in range(4):
                units.append(lambda c=c: fm_proj(hTb, wq, 512 + c * 128, 8, Qst.ap()[:, c, gg * 512:(gg + 1) * 512], [Qst.sl(c)], scale=0.125))
        for t in range(4):
            vt = (vb0 + (t // 2) * 2) * 2 + (t % 2)

            def uv(t=t, vt=vt):
                b = next_bank()

                def mm(h):
                    r = None
                    for k in range(8):
                        r = h.matmul(PS[b].ap(), lhsT=hTb.ap()[:, k, t * 128:(t + 1) * 128], rhs=wkv.ap()[:, k, 640:1152],
                                     start=(k == 0), stop=(k == 7))
                    return r
                S.add('pe', mm, [hTb, wkv], [PS[b]])
                evac(Vb.ap()[:, vt, :, 0:64], PS[b].ap().rearrange("p (h d) -> p h d", h=8), [PS[b]], [Vb.sl(vt)])
            units.append(uv)

            def uv2(t=t, vt=vt):
                b2 = next_bank()

                def mm2(h):
                    r = None
                    for k in range(8):
                        r = h.matmul(PS[b2].ap()[:, 0:128], lhsT=hTb.ap()[:, k, t * 128:(t + 1) * 128], rhs=wkv.ap()[:, k, 1152:1280],
                                     start=(k == 0), stop=(k == 7))
                    return r
                S.add('pe', mm2, [hTb, wkv], [PS[b2]])
                evac(va.ap()[:, vt, :, 0:64], PS[b2].ap()[:, 0:128].rearrange("p (h d) -> p h d", h=2), [PS[b2]], [va.sl(vt)])
            units.append(uv2)
        return units

    order = [4, 5, 6, 7, 0, 1, 2, 3]
    fe0 = phase1_frontend(order[0], 0)
    for e_, t_ in fe0[0:3]:
        e_()
    fe0[0][1]()
    fe0[3][0]()
    fe0[1][1]()
    fe0[2][1]()
    fe0[3][1]()
    emit_t5()
    for i, g in enumerate(order):
        units = phase1_proj(g, i)
        n = len(units)
        extras = []
        if i + 1 < len(order):
            fes = phase1_frontend(order[i + 1], i + 1)
            for t, (e_, t_) in enumerate(fes):
                p0 = (t * n) // 4
                extras.append((p0, e_))
                extras.append((p0 + 5, t_))
        interleave(units, extras)

    def dump(ap, ncols, parts=128, col0=0):
        stg = sb("dbgstg", [128, 2048], F32, SB_LIMIT - 8192 - 32)
        for c0 in range(0, ncols, 2048):
            w = min(2048, ncols - c0)
            S.add('dve', lambda h, c0=c0, w=w: h.tensor_copy(out=stg.ap()[0:parts, 0:w], in_=ap[:, c0:c0 + w]),
                  [Buf(None, 'sb', 0, SB_LIMIT, [1], F32)], [stg])
            dma('sp', 'dbg', dbg_d.ap()[0:parts, col0 + c0:col0 + c0 + w], stg.ap()[0:parts, 0:w], [stg], [dbg_d])

    def finish():
        S.add('sp', lambda h: None, [out_d] + ([dbg_d] if dbg_d is not None else []), [])
        info = S.emit()
        return nc, info

    if debug == 'p1':
        dump(Qst.ap()[:, 0, :], 2048, col0=0)
        dump(Kst.ap()[:, 0, :], 4096, col0=2048)
        dump(Vb.ap()[:, 3, :, :].rearrange("p h d -> p (h d)"), 520, col0=6144)
        dump(qaT.ap()[:, 1, 0:512], 512, col0=6144 + 520)
        dump(kaT.ap()[:, 0:512], 512, col0=6144 + 1032)
        return finish()

    Oa = sb("Oa", [128, 16, 512], BF16, O_W1)
    Ob = sb("Ob", [128, 16, 512], BF16, O_W1 + 16384)
    O_X = O_W1 + 32768
    Wa = sb("Wa", [128, 8, LW_A], BF16, O_X)
    Wa2 = sb("Wa2", [128, 8, 128], BF16, O_X + 6144)
    Sp = [sb("Sp", [128, 512], F32, O_X + 8192 + i * 2048) for i in range(3)]
    Pt = [sb("Pt", [128, 512], BF16, O_X + 14336 + i * 1024) for i in range(3)]
    den = sb("den", [128, 16], F32, O_X + 17408)
    gm = sb("gm", [128, 16, 16], F32, O_X + 17472)
    mx = sb("mx", [128, 16, 8], F32, O_X + 18496)
    selb = sb("selb", [128, 16, 16], F32, O_X + 19008)
    Mtok = sb("Mtok", [128, 16, 16], BF16, O_X + 20032)
    ksum = sb("ksum", [128, 4, 16], F32, O_X + 20544)
    khi = sb("khi", [128, 4, 16], BF16, O_X + 20800)
    klo = sb("klo", [128, 4, 16], BF16, O_X + 20928)
    ktmp = sb("ktmp", [128, 4, 16], F32, O_X + 21056)
    Wm = [sb("Wm", [128, LW_M], BF16, O_X + 21312 + i * 5120) for i in range(2)]
    O_X2 = O_X + 31552
    SpA = [sb("SpA", [128, 512], F32, O_X2 + i * 2048) for i in range(4)]
    PtA = [sb("PtA", [128, 512], BF16, O_X2 + 8192 + i * 1024) for i in range(4)]
    denA = [sb("denA", [128, 16], F32, O_X2 + 12288 + i * 64) for i in range(4)]
    Kaug = [sb("Kaug", [80, 4096], BF16, O_SWA + i * 8192) for i in range(2)]
    Qaug = [sb("Qaug", [80, 2048], BF16, O_SWA + 16384 + i * 4096) for i in range(2)]

    for hh in range(8):
        src = bass.AP(scra_d.h, hh * 128 * LT_A + 127, [[LT_A - 1, 128], [1, LW_A]])
        dma('sp', f'c_wa{hh % 4}', Wa.ap()[:, hh, :], src, [scra_d], [Wa.sl(hh)])
    S.add('dve', lambda h: h.tensor_scalar(out=Wa2.ap(), in0=Wa.ap()[:, :, 256:384], scalar1=flag.ap()[:, 1:2], scalar2=None,
                                           op0=ALU.add), [Wa, flag], [Wa2])

    swa_its = [(qt, g) for qt in range(16) for g in range(2)]

    def swa_bufs(it):
        sbank = [PS[0], PS[1]] if it % 2 == 0 else [PS[4], PS[5]]
        obank = PS[2 + (it % 2)]
        sp_ = [SpA[2 * (it % 2)], SpA[2 * (it % 2) + 1]]
        pt_ = [PtA[2 * (it % 2)], PtA[2 * (it % 2) + 1]]
        return sbank, obank, sp_, pt_, denA[it % 2]

    def swa_front(it):
        qt, g = swa_its[it]
        s = qt // 2
        vt = (2 * s + 1) * 2 + (qt % 2)
        sbank, obank, sp_, pt_, dnb = swa_bufs(it)
        for j, kt in enumerate((vt - 1, vt)):
            S.add('pe', lambda h, j=j, kt=kt: h.matmul(
                sbank[j].ap(), lhsT=kaT.ap()[64 * g:64 * g + 64, kt * 128:(kt + 1) * 128],
                rhs=qaT.ap()[64 * g:64 * g + 64, :, qt * 128:(qt + 1) * 128], start=True, stop=True),
                [kaT, qaT], [sbank[j]])
            if j == 0:
                tab = Wa2.ap()[:, 4 * g:4 * g + 4, :] if qt == 0 else Wa.ap()[:, 4 * g:4 * g + 4, 256:384]
            else:
                tab = Wa.ap()[:, 4 * g:4 * g + 4, 128:256]
            S.add('dve', lambda h, j=j, tab=tab: h.tensor_tensor(
                out=sp_[j].ap().rearrange("p (a q) -> p a q", a=4), in0=sbank[j].ap().rearrange("p (a q) -> p a q", a=4),
                in1=tab, op=ALU.add), [sbank[j], Wa, Wa2], [sp_[j]])
            S.add('act', lambda h, j=j: h.activation(out=pt_[j].ap(), in_=sp_[j].ap(), func=AF.Exp), [sp_[j]], [pt_[j]])

    def swa_back(it):
        qt, g = swa_its[it]
        s = qt // 2
        vt = (2 * s + 1) * 2 + (qt % 2)
        sbank, obank, sp_, pt_, dnb = swa_bufs(it)
        ov = obank.ap()[:, 0:260].rearrange("p (a d) -> p a d", a=4)

        def pv(h):
            r = None
            for a in range(4):
                for j, kt in enumerate((vt - 1, vt)):
                    r = h.matmul(ov[:, a, :], lhsT=pt_[j].ap()[:, a * 128:(a + 1) * 128], rhs=va.ap()[:, kt, g, :],
                                 start=(j == 0), stop=(j == 1))
            return r
        S.add('pe', pv, [pt_[0], pt_[1], va], [obank])
        S.add('dve', lambda h: h.tensor_tensor(out=dnb.ap()[:, 0:4], in0=ov[:, :, 64], in1=esink.ap()[:, 4 * g:4 * g + 4],
                                               op=ALU.add), [obank, esink], [dnb])
        S.add('dve', lambda h: h.reciprocal(out=dnb.ap()[:, 4:8], in_=dnb.ap()[:, 0:4]), [dnb], [dnb])
        S.add('dve', lambda h: h.tensor_tensor(
            out=Oa.ap()[:, qt, 256 * g:256 * g + 256].rearrange("p (a d) -> p a d", a=4), in0=ov[:, :, 0:64],
            in1=dnb.ap()[:, 4:8].unsqueeze(2).to_broadcast([128, 4, 64]), op=ALU.mult), [obank, dnb], [Oa.sl(qt)])


    S.add('dve', lambda h: h.tensor_reduce(out=ksum.ap().rearrange("p c b -> p (c b)"),
                                           in_=Kst.ap().rearrange("p c (b k) -> p (c b) k", k=256), op=ALU.add, axis=AX.X),
          [Kst], [ksum])
    S.add('dve', lambda h: h.tensor_copy(out=khi.ap(), in_=ksum.ap()), [ksum], [khi])
    S.add('dve', lambda h: h.tensor_tensor(out=ktmp.ap(), in0=ksum.ap(), in1=khi.ap(), op=ALU.subtract), [ksum, khi], [ktmp])
    S.add('dve', lambda h: h.tensor_copy(out=klo.ap(), in_=ktmp.ap()), [ktmp], [klo])

    def moba_head_prep(hd):
        c, half = hd // 2, hd % 2
        pr = slice(64 * half, 64 * half + 64)
        ka_, qa_, wm_ = Kaug[hd % 2], Qaug[hd % 2], Wm[hd % 2]
        pbk = {}

        def stA2():
            ce = 'dve'
            S.add(ce, lambda h: h.tensor_copy(out=ka_.ap()[0:64, :], in_=Kst.ap()[pr, c, :]), [Kst], [ka_])
            S.add(ce, lambda h: h.tensor_copy(out=qa_.ap()[0:64, :], in_=Qst.ap()[pr, c, :]), [Qst], [qa_])

        def stA():
            stA2()
            stA1()

        def stA1():
            gb_ = sbank_next()
            gv = gb_.ap()[:, 0:256].rearrange("p (t b) -> p t b", t=16)
            src = bass.AP(scrm_d.h, hd * 128 * LT_M + 127, [[LT_M - 1, 128], [1, LW_M]])
            dma('sp', f'wm{hd % 2}', wm_.ap(), src, [scrm_d], [wm_])

            def gmm(h):
                r = None
                for qt in range(16):
                    r = h.matmul(gv[:, qt, :], lhsT=Qst.ap()[pr, c, qt * 128:(qt + 1) * 128], rhs=khi.ap()[pr, c, :], start=True, stop=False)
                    r = h.matmul(gv[:, qt, :], lhsT=Qst.ap()[pr, c, qt * 128:(qt + 1) * 128], rhs=klo.ap()[pr, c, :], start=False, stop=True)
                return r
            S.add('pe', gmm, [Qst, khi, klo], [gb_])
            S.add('dve', lambda h: h.tensor_tensor(out=gm.ap(), in0=gv, in1=pm.ap(), op=ALU.add), [gb_, pm], [gm])
            for qt in range(16):
                S.add('dve', lambda h, qt=qt: h.max(out=mx.ap()[:, qt, :], in_=gm.ap()[:, qt, :]), [gm], [mx])
            S.add('dve', lambda h: h.tensor_scalar(out=mx.ap()[:, :, 2:3], in0=mx.ap()[:, :, 2:3], scalar1=-1e30, scalar2=None,
                                                   op0=ALU.max), [mx], [mx])
            S.add('dve', lambda h: h.tensor_tensor(out=selb.ap(), in0=gm.ap(), in1=mx.ap()[:, :, 2:3].to_broadcast([128, 16, 16]),
                                                   op=ALU.is_ge), [gm, mx], [selb])
            S.add('dve', lambda h: h.scalar_tensor_tensor(out=Mtok.ap(), in0=selb.ap(), scalar=-NEG, in1=basem.ap(),
                                                          op0=ALU.mult, op1=ALU.add), [selb, basem], [Mtok])

        def stB(half2):
            tb_ = sbank_next()
            pbk[half2] = tb_
            pvv = tb_.ap().bitcast(BF16)

            def tr(h):
                r = None
                for t8 in range(8):
                    qt = half2 * 8 + t8
                    r = h.transpose(out=pvv[0:16, t8 * 128:(t8 + 1) * 128], in_=Mtok.ap()[:, qt, :], identity=ident.ap())
                return r
            S.add('pe', tr, [Mtok, ident], [tb_])

        def stC(half2):
            tb_ = pbk[half2]
            pvv = tb_.ap().bitcast(BF16)
            S.add('act', lambda h: h.copy(out=qa_.ap()[64:80, half2 * 1024:(half2 + 1) * 1024], in_=pvv[0:16, 0:1024]), [tb_], [qa_])
        return [stA, lambda: (stB(0), stC(0)), lambda: (stB(1), stC(1)), stA1, stA2]

    iters = []
    for hd in range(8):
        for s in range(8):
            nblk = 2 * s + 2
            for j in range(nblk):
                iters.append((hd, s, j, j == 0, j == nblk - 1))

    def it_far(itx):
        hd, s, j, first, last = iters[itx]
        return (2 * s + 1 - j) * 256 - 128 >= 2176

    def it_pebias(itx):
        return (not it_far(itx)) and (itx % PEB_MOD != 0)

    SB4 = [PS[0], PS[1], PS[2], PS[7]]
    srot = [0]
    it_bank = {}

    def sbank_next():
        return PS[7]

    def emit_S(itx):
        hd, s, j, first, last = iters[itx]
        ka_, qa_, wm_ = Kaug[hd % 2], Qaug[hd % 2], Wm[hd % 2]
        bank = PS[itx % 3]
        it_bank[itx] = bank
        delta0 = (2 * s + 1 - j) * 256
        peb = it_pebias(itx)

        def mm(h):
            r = None
            for kk in range(2):
                kt = 2 * j + (1 - kk)
                r = h.matmul(bank.ap()[:, kk * 256:(kk + 1) * 256], lhsT=ka_.ap()[0:80, kt * 128:(kt + 1) * 128],
                             rhs=qa_.ap()[0:80, s * 256:(s + 1) * 256], start=True, stop=not peb)
                if peb:
                    o_ = delta0 + 128 * kk
                    r = h.matmul(bank.ap()[:, kk * 256:(kk + 1) * 256], lhsT=ident.ap(), rhs=wm_.ap()[:, o_:o_ + 256], start=False, stop=True)
            return r
        S.add('pe', mm, [ka_, qa_] + ([wm_, ident] if peb else []), [bank])

    pend_norm = []

    def emit_rest(itx):
        hd, s, j, first, last = iters[itx]
        wm_ = Wm[hd % 2]
        bank = it_bank.pop(itx)
        sp_ = SpA[itx % 4]
        pt_ = PtA[itx % 4]
        own = 2 * s + 1
        delta0 = (own - j) * 256
        if it_far(itx):
            S.add('act', lambda h: h.activation(out=pt_.ap(), in_=bank.ap(), func=AF.Exp, bias=rb31.ap()[:, 8 + hd:9 + hd], scale=1.0),
                  [bank, rb31], [pt_])
        elif it_pebias(itx):
            S.add('act', lambda h: h.activation(out=pt_.ap(), in_=bank.ap(), func=AF.Exp), [bank], [pt_])
        else:
            w = wm_.ap()
            in1 = bass.AP(w.tensor, w.offset + delta0, [list(w.ap[0]), [128, 2], [1, 256]])
            S.add('dve', lambda h: h.tensor_tensor(out=sp_.ap().rearrange("p (k q) -> p k q", k=2),
                                                   in0=bank.ap().rearrange("p (k q) -> p k q", k=2), in1=in1, op=ALU.add),
                  [bank, wm_], [sp_])
            S.add('act', lambda h: h.activation(out=pt_.ap(), in_=sp_.ap(), func=AF.Exp), [sp_], [pt_])
        par_ = (hd * 8 + s) % 2
        ob = [PS[3], PS[4]] if par_ == 0 else [PS[5], PS[6]]
        dn_ = denA[2 + par_]

        def pv(h):
            r = None
            for kk in range(2):
                kt = 2 * j + (1 - kk)
                for q2 in range(2):
                    r = h.matmul(ob[q2].ap()[:, 0:65], lhsT=pt_.ap()[:, kk * 256 + q2 * 128:kk * 256 + q2 * 128 + 128],
                                 rhs=Vb.ap()[:, kt, hd, :], start=(first and kk == 0), stop=(last and kk == 1))
            return r
        S.add('pe', pv, [pt_, Vb], [ob[0], ob[1]])
        while pend_norm and pend_norm[0][0] <= itx:
            pend_norm.pop(0)[1]()
        if last:
            def norm():
                for q2 in range(2):
                    qt = 2 * s + q2
                    S.add('dve', lambda h, q2=q2: h.reciprocal(out=dn_.ap()[:, 8 + q2:9 + q2], in_=ob[q2].ap()[:, 64:65]), [ob[q2]], [dn_])
                    S.add('dve', lambda h, q2=q2, qt=qt: h.tensor_scalar(out=Ob.ap()[:, qt, hd * 64:(hd + 1) * 64], in0=ob[q2].ap()[:, 0:64],
                                                                         scalar1=dn_.ap()[:, 8 + q2:9 + q2], scalar2=None, op0=ALU.mult),
                          [ob[q2], dn_], [Ob.sl(qt)])
            n_next = 2 * (s + 1) + 2 if s < 7 else 2
            pend_norm.append((itx + min(4, n_next), norm))

    LOOK = 2
    n_it = len(iters)
    prep0 = moba_head_prep(0)
    swa_front(0)
    for it in range(len(swa_its)):
        if it == 12:
            prep0[3]()
        if it + 1 < len(swa_its):
            swa_front(it + 1)
        swa_back(it)

    if debug == 'p2a':
        dump(Oa.ap().rearrange("p t c -> p (t c)"), 8192)
        return finish()

    for i in range(2):
        dma('sp', f'c_blkoh{i}', Kaug[i].ap()[64:80, :], blkoh_d.ap(), [blkoh_d], [Kaug[i]])
    prep0[4]()
    prep0[1]()
    prep0[2]()
    events = {}

    def at(itx_, f):
        events.setdefault(itx_, []).append(f)

    bgb = [sb("bgb", [128, 2048], F32, o_) for o_ in (183744, 196288, 204480)]
    ev_i = 10
    for pass_ in (1, 2):
        ld_, ac_, fin_ = mod_pass(pass_ * 2048, 2048, bgb[0:2], bgb[2], f'mp{pass_}', lambda: PS[7], eng='pool', dq='pool')
        at(ev_i, lambda ld_=ld_: (ld_(0), ld_(1)))
        for k in range(9):
            def stp(k=k, ld_=ld_, ac_=ac_):
                ac_(k)
                if k + 2 < 9:
                    ld_(k + 2)
            at(ev_i + 12 * (k + 1), stp)
        at(ev_i + 12 * 10 + 60, fin_)
        ev_i += 12 * 10 + 70

    wgate = sb("wgate", [128, 8, 2048], BF16, 4096)
    wbr = sb("wbr", [128, 8, D], BF16, 70144)

    def prefetch_3a_weights():
        load_w(wbr, wba_d.ap().rearrange("(k p) n -> p k n", p=128), D, 'w_br', wba_d, kchunks=4, dst_k0=0)
        load_w(wbr, wbb_d.ap().rearrange("(k p) n -> p k n", p=128), D, 'w_br', wbb_d, kchunks=4, dst_k0=4)
        load_w(wgate, wg_d.ap().rearrange("(k p) n -> p k n", p=128), 2048, 'w_gate', wg_d)

    for itx in range(n_it + LOOK):
        if itx < n_it:
            hd, s_, j_ = iters[itx][0], iters[itx][1], iters[itx][2]
            if hd == 6 and s_ == 5 and j_ == 0:
                at(itx, prefetch_3a_weights)
            if s_ == 4 and j_ == 0 and hd + 1 < 8:
                st = moba_head_prep(hd + 1)
                for d_, f_ in zip((0, 16, 26), st):
                    at(itx + d_, f_)
            for f_ in events.pop(itx, []):
                f_()
            emit_S(itx)
        if itx - LOOK >= 0:
            emit_rest(itx - LOOK)
    while pend_norm:
        pend_norm.pop(0)[1]()
    assert not events, sorted(events)

    if debug in ('p2b', 'p2b_nolate'):
        dump(Ob.ap().rearrange("p t c -> p (t c)"), 8192)
        return finish()

    mergedT = sb("mergedT", [128, 8, 2048], BF16, 36864)
    OT = [sb("OT", [128, 8, 512], BF16, 86528 + i * 8192) for i in range(2)]
    sig = [sb("sig", [128, 512], F32, 102912 + i * 2048) for i in range(8)]
    assert 119296 <= O_W1
    O_F3 = O_W1 + 32768
    G1bc3 = sb("G1bc3", [128, D], F32, O_F3)
    SH1bc3 = sb("SH1bc3", [128, D], F32, O_F3 + 4096)
    xt3 = [sb("xt3", [128, D], F32, O_F3 + 8192 + i * 4096) for i in range(2)]
    hb3 = [sb("hb3", [128, D], BF16, O_F3 + 16384 + i * 2048) for i in range(2)]
    hT3 = [sb("hT3", [128, 8, 512], BF16, O_F3 + 20480 + i * 8192) for i in range(2)]
    g1bc3 = sb("g1bc3", [128, D], F32, O_F3 + 36864)
    load_mod_bc(SH1bc3, 0, 'c_sh1')
    load_mod_bc(G1bc3, 1, 'c_g1a')
    dma('sp', 'c_g1b', g1bc3.ap(), bcast_rows(g1_d.ap()), [g1_d], [g1bc3])
    S.add('dve', lambda h: h.scalar_tensor_tensor(out=G1bc3.ap(), in0=G1bc3.ap(), scalar=1.0, in1=g1bc3.ap(),
                                                  op0=ALU.add, op1=ALU.mult), [G1bc3, g1bc3], [G1bc3])
    wout = sb("wout", [128, 8, D], BF16, 193152)
    load_w(wout, wout_d.ap().rearrange("(k p) n -> p k n", p=128), D, 'w_out', wout_d)

    def p3_frontend(g):
        res = []
        for t in range(4):
            tile_i = g * 4 + t
            e_, t_ = frontend(x_own.ap()[tile_i * 128:(tile_i + 1) * 128, :], [x_own], xt3[tile_i % 2], hb3[tile_i % 2], hT3[g % 2], t,
                              G1bc3, SH1bc3, 0, xkey=f'xt3{tile_i % 2}')

            def tr2(t_=t_, tile_i=tile_i, t=t, g=g):
                t_()
                pvv = ps_bf(1).rearrange("p (c t) -> p c t", c=8)

                def tr(h):
                    r = None
                    for cc in range(4):
                        r = h.transpose(out=pvv[:, cc, :], in_=Oa.ap()[:, tile_i, cc * 128:(cc + 1) * 128], identity=ident.ap())
                    for cc in range(4):
                        r = h.transpose(out=pvv[:, 4 + cc, :], in_=Ob.ap()[:, tile_i, cc * 128:(cc + 1) * 128], identity=ident.ap())
                    return r
                S.add('pe', tr, [Oa.sl(tile_i), Ob.sl(tile_i), ident], [PS[1]])
                S.add('dve', lambda h: h.tensor_copy(out=OT[g % 2].ap()[:, :, t * 128:(t + 1) * 128], in_=pvv), [PS[1]], [OT[g % 2]])
            res.append((e_, tr2))
        return res

    p3_rr = [0]

    def p3_bank():
        b_ = PS[2 + (p3_rr[0] % 6)]
        p3_rr[0] += 1
        return b_

    def p3_units(g):
        hTb, OTb = hT3[g % 2], OT[g % 2]
        units = []
        for oc in range(8):
            def unit(oc=oc):
                sg = sig[4 * (oc % 2):4 * (oc % 2) + 4]
                b0, b1, b2, b3 = p3_bank(), p3_bank(), p3_bank(), p3_bank()

                def mm_a(h):
                    r = None
                    for k in range(4):
                        r = h.matmul(b0.ap(), lhsT=wbr.ap()[:, k, oc * 128:(oc + 1) * 128], rhs=OTb.ap()[:, k, :], start=(k == 0), stop=(k == 3))
                    return r

                def mm_b(h):
                    r = None
                    for k in range(4):
                        r = h.matmul(b1.ap(), lhsT=wbr.ap()[:, 4 + k, oc * 128:(oc + 1) * 128], rhs=OTb.ap()[:, 4 + k, :], start=(k == 0), stop=(k == 3))
                    return r

                def mm_ga(h):
                    r = None
                    for k in range(8):
                        r = h.matmul(b2.ap(), lhsT=wgate.ap()[:, k, oc * 128:(oc + 1) * 128], rhs=hTb.ap()[:, k, :], start=(k == 0), stop=(k == 7))
                    return r

                def mm_gb(h):
                    r = None
                    for k in range(8):
                        r = h.matmul(b3.ap(), lhsT=wgate.ap()[:, k, D + oc * 128:D + (oc + 1) * 128], rhs=hTb.ap()[:, k, :], start=(k == 0), stop=(k == 7))
                    return r
                S.add('pe', mm_ga, [wgate, hTb], [b2])
                S.add('act', lambda h: h.activation(out=sg[0].ap(), in_=b2.ap(), func=AF.Sigmoid), [b2], [sg[0]])
                S.add('pe', mm_gb, [wgate, hTb], [b3])
                S.add('act', lambda h: h.activation(out=sg[1].ap(), in_=b3.ap(), func=AF.Sigmoid), [b3], [sg[1]])
                S.add('pe', mm_a, [wbr, OTb], [b0])
                S.add('dve', lambda h: h.tensor_tensor(out=sg[2].ap(), in0=b0.ap(), in1=sg[0].ap(), op=ALU.mult), [b0, sg[0]], [sg[2]])
                S.add('pe', mm_b, [wbr, OTb], [b1])
                S.add('dve', lambda h: h.tensor_tensor(out=sg[3].ap(), in0=b1.ap(), in1=sg[1].ap(), op=ALU.mult), [b1, sg[1]], [sg[3]])
                S.add('pool', lambda h: h.tensor_tensor(out=mergedT.ap()[:, oc, g * 512:(g + 1) * 512], in0=sg[2].ap(), in1=sg[3].ap(),
                                                        op=ALU.add), [sg[2], sg[3]], [mergedT.sl(oc)])
            units.append(unit)
        return units

    for e_, t_ in p3_frontend(0):
        e_()
        t_()
    x1 = sb("x1", [128, 16, D], F32, O_W1)
    X1_EARLY = (8, 9, 10, 11, 12)

    def early_x1():
        for t in X1_EARLY:
            dma('sp', f'x1ld{t % 4}', x1.ap()[:, t, :], x_own.ap()[t * 128:(t + 1) * 128, :], [x_own], [x1.sl(t)])

    for g in range(4):
        units = p3_units(g)
        extras = []
        if g == 3:
            extras.append((1, early_x1))
        if g + 1 < 4:
            for t, (e_, t_) in enumerate(p3_frontend(g + 1)):
                extras.append((2 * t, e_))
                extras.append((2 * t + 1, t_))
        interleave(units, extras)

    h2T = sb("h2T", [128, 8, 2048], BF16, 4096)
    WB = (69632, 86016, 36864, 53248)
    wmi = [sb("wmi", [128, 8, 512], BF16, o_) for o_ in WB]
    wmo = [sb("wmo", [128, 4, D], BF16, o_ + 8192) for o_ in WB]
    aT = [sb("aT", [128, 4, 512], BF16, 102400 + i * 4096) for i in range(2)]
    O_G = O_W1 + 65536
    gbc = sb("gbc", [128, D], F32, O_G)
    tmpz = [sb("tmpz", [128, 512], F32, O_G + 4096 + i * 2048) for i in range(2)]
    rbuf = [sb("rbuf", [128, 512], F32, O_G + 8192 + i * 2048) for i in range(2)]
    O_4 = 102400
    O_6 = 197248
    G2bc = sb("G2bc", [128, D], F32, O_4)
    SH2bc = sb("SH2bc", [128, D], F32, O_4 + 4096)
    t4 = sb("t4", [128, D], F32, O_4 + 8192)
    hb4s = [sb("hb4", [128, D], BF16, O_4 + 12288), sb("hb4", [128, D], BF16, O_G + 4096)]
    wmi_v = wmi_d.ap().rearrange("(k p) n -> p k n", p=128)
    wmo_v = wmo_d.ap().rearrange("(k p) n -> p k n", p=128)

    def load_piece(p):
        load_w(wmi[p % 4], wmi_v[:, :, p * 512:(p + 1) * 512], 512, f'w_mi{p % 4}', wmi_d)
        load_w(wmo[p % 4], wmo_v[:, p * 4:(p + 1) * 4, :], D, f'w_mo{p % 4}', wmo_d, kchunks=4)

    load_mod_bc(gbc, 2, 'c_gate')
    for k in range(8):
        S.add('pool', lambda h, k=k: h.tensor_tensor(out=wout.ap()[:, k, :], in0=wout.ap()[:, k, :], in1=gbc.ap(), op=ALU.mult),
              [wout.sl(k), gbc], [wout.sl(k)])
    load_mod_bc(SH2bc, 3, 'c_sh2')
    load_mod_bc(G2bc, 4, 'c_g2a')
    dma('sp', 'c_g2b', t4.ap(), bcast_rows(g2_d.ap()), [g2_d], [t4])
    S.add('dve', lambda h: h.scalar_tensor_tensor(out=G2bc.ap(), in0=G2bc.ap(), scalar=1.0, in1=t4.ap(),
                                                  op0=ALU.add, op1=ALU.mult), [G2bc, t4], [G2bc])
    for t in range(16):
        if t not in X1_EARLY:
            dma('sp', f'x1ld{t % 4}', x1.ap()[:, t, :], x_own.ap()[t * 128:(t + 1) * 128, :], [x_own], [x1.sl(t)])

    def p4_elem(t):
        xr = x1.sl(t)
        x_ap = x1.ap()[:, t, :]
        st_ = stat2[t % 2]
        hb4 = hb4s[t % 2]
        S.add('act', lambda h: h.activation(out=hb4.ap(), in_=x_ap, func=AF.Square, accum_out=st_.ap()[:, 0:1]), [xr], [hb4, st_])
        S.add('act', lambda h: h.activation(out=st_.ap()[:, 1:2], in_=st_.ap()[:, 0:1], func=AF.Sqrt,
                                            bias=epsb.ap()[:, 0:1], scale=1.0 / D), [st_, epsb], [st_])
        S.add('dve', lambda h: h.reciprocal(out=st_.ap()[:, 2:3], in_=st_.ap()[:, 1:2]), [st_], [st_])
        S.add('dve', lambda h: h.scalar_tensor_tensor(out=t4.ap(), in0=x_ap, scalar=st_.ap()[:, 2:3], in1=G2bc.ap(),
                                                      op0=ALU.mult, op1=ALU.mult), [xr, st_, G2bc], [t4])
        S.add('pool', lambda h: h.tensor_tensor(out=hb4.ap(), in0=t4.ap(), in1=SH2bc.ap(), op=ALU.add), [t4, SH2bc], [hb4])

    def p4_tr(t):
        hb4 = hb4s[t % 2]
        pvv = ps_bf(4).rearrange("p (c t) -> p c t", c=8)

        def tr(h):
            r = None
            for c in range(8):
                r = h.transpose(out=pvv[:, c, :], in_=hb4.ap()[:, c * 128:(c + 1) * 128], identity=ident.ap())
            return r
        S.add('pe', tr, [hb4, ident], [PS[4]])
        S.add('act', lambda h: h.copy(out=h2T.ap()[:, :, t * 128:(t + 1) * 128], in_=pvv), [PS[4]], [h2T])

    zc = 0
    for t in range(16):
        if t == 4:
            load_piece(0)
        for nh in range(2):
            b = PS[zc % 4]
            zc += 1

            def mm(h, t=t, nh=nh, b=b):
                r = None
                for k in range(8):
                    r = h.matmul(b.ap(), lhsT=mergedT.ap()[:, k, t * 128:(t + 1) * 128], rhs=wout.ap()[:, k, nh * 512:(nh + 1) * 512],
                                 start=(k == 0), stop=(k == 7))
                return r
            S.add('pe', mm, [mergedT, wout], [b])
            S.add('dve', lambda h, t=t, nh=nh, b=b: h.tensor_tensor(out=x1.ap()[:, t, nh * 512:(nh + 1) * 512], in0=b.ap(),
                                                                    in1=x1.ap()[:, t, nh * 512:(nh + 1) * 512], op=ALU.add),
                  [b, x1.sl(t)], [x1.sl(t)])
        if t >= 1:
            p4_elem(t - 1)
        if t >= 2:
            p4_tr(t - 2)
    p4_elem(15)
    p4_tr(14)
    p4_tr(15)

    if debug == 'p3':
        dump(x1.ap()[:, 0:8, :].rearrange("p t c -> p (t c)"), 8192)
        return finish()

    gbc2 = sb("gbc2", [128, D], F32, O_G)
    load_mod_bc(gbc2, 5, 'c_gate')
    gfbc = sb("gfbc", [128, D], F32, O_6)
    ot = [sb("ot", [128, D], F32, O_6 + 4096 + i * 4096) for i in range(2)]
    junk6 = sb("junk6", [128, D], BF16, O_6 + 12288)

    def p6_tile(t):
        xr = x1.sl(t)
        x_ap = x1.ap()[:, t, :]
        o_ = ot[t % 2]
        st_ = stat2[t % 2]
        S.add('act', lambda h: h.activation(out=junk6.ap(), in_=x_ap, func=AF.Square, accum_out=st_.ap()[:, 0:1]), [xr], [junk6, st_])
        S.add('act', lambda h: h.activation(out=st_.ap()[:, 1:2], in_=st_.ap()[:, 0:1], func=AF.Sqrt,
                                            bias=epsb.ap()[:, 0:1], scale=1.0 / D), [st_, epsb], [st_])
        S.add('dve', lambda h: h.reciprocal(out=st_.ap()[:, 2:3], in_=st_.ap()[:, 1:2]), [st_], [st_])
        S.add('dve', lambda h: h.scalar_tensor_tensor(out=o_.ap(), in0=x_ap, scalar=st_.ap()[:, 2:3], in1=gfbc.ap(),
                                                      op0=ALU.mult, op1=ALU.mult), [xr, st_, gfbc], [o_])
        dma('sp', f'ost{t % 2}', out_d.ap()[t * 128:(t + 1) * 128, :], o_.ap(), [o_], [out_d.el(t * 128 * D, (t + 1) * 128 * D)])

    yc = 0
    uc = 0

    def fold_gate2(p):
        wo = wmo[p % 4]
        for k in range(4):
            S.add('pool', lambda h, k=k: h.tensor_tensor(out=wo.ap()[:, k, :], in0=wo.ap()[:, k, :], in1=gbc2.ap(), op=ALU.mult),
                  [wo.sl(k), gbc2], [wo.sl(k)])

    fold_gate2(0)
    load_piece(1)
    fold_gate2(1)
    load_piece(2)
    fold_gate2(2)
    NPC = 8
    pg = [(p, g) for p in range(NPC) for g in range(4)]

    def emit_U(i):
        p, g = pg[i]
        nonlocal_uc = uc_box
        wi = wmi[p % 4]
        aTb = aT[i % 2]
        for hc in range(4):
            b = PS[4 + (nonlocal_uc[0] % 4)]
            rb_ = rbuf[nonlocal_uc[0] % 2]
            nonlocal_uc[0] += 1

            def mm(h, hc=hc, b=b):
                r = None
                for k in range(8):
                    r = h.matmul(b.ap(), lhsT=wi.ap()[:, k, hc * 128:(hc + 1) * 128], rhs=h2T.ap()[:, k, g * 512:(g + 1) * 512],
                                 start=(k == 0), stop=(k == 7))
                return r
            S.add('pe', mm, [wi, h2T], [b])
            S.add('act', lambda h, b=b, rb_=rb_: h.activation(out=rb_.ap(), in_=b.ap(), func=AF.Relu), [b], [rb_])
            S.add('act', lambda h, hc=hc, rb_=rb_: h.activation(out=aTb.ap()[:, hc, :], in_=rb_.ap(), func=AF.Square),
                  [rb_], [aTb.sl(hc)])

    def emit_Y(i):
        p, g = pg[i]
        wo = wmo[p % 4]
        aTb = aT[i % 2]
        if g == 0:
            if p + 3 < NPC:
                load_piece(p + 3)
                fold_gate2(p + 3)
            if p == NPC - 1:
                dma('sp', 'c_gf', gfbc.ap(), bcast_rows(gf_d.ap()), [gf_d], [gfbc])
        for t in range(4):
            tt = g * 4 + t
            for nh in range(2):
                b = PS[yc_box[0] % 4]
                yc_box[0] += 1

                def mm(h, t=t, nh=nh, b=b):
                    r = None
                    for k in range(4):
                        r = h.matmul(b.ap(), lhsT=aTb.ap()[:, k, t * 128:(t + 1) * 128], rhs=wo.ap()[:, k, nh * 512:(nh + 1) * 512],
                                     start=(k == 0), stop=(k == 3))
                    return r
                S.add('pe', mm, [aTb, wo], [b])
                S.add('dve', lambda h, tt=tt, nh=nh, b=b: h.tensor_tensor(out=x1.ap()[:, tt, nh * 512:(nh + 1) * 512], in0=b.ap(),
                                                                          in1=x1.ap()[:, tt, nh * 512:(nh + 1) * 512], op=ALU.add),
                      [b, x1.sl(tt)], [x1.sl(tt)])
            if p == NPC - 1:
                p6_tile(tt)

    uc_box, yc_box = [0], [0]
    emit_U(0)
    for i in range(len(pg)):
        if i + 1 < len(pg):
            emit_U(i + 1)
        emit_Y(i)
    return finish()


def _consts():
    ident = np.eye(128, dtype=np.float32).astype(ml_dtypes.bfloat16)
    ohm = np.zeros((33, LT_M), np.float32)
    d = np.arange(LT_M) - 255
    bk = t5_bucket_np(d)
    for i in range(LT_M):
        if d[i] < 0:
            ohm[32, i] = 1.0
        else:
            ohm[bk[i], i] = 1.0
    oha = np.zeros((33, LT_A), np.float32)
    d = np.arange(LT_A) - 255
    bk = t5_bucket_np(d)
    for i in range(LT_A):
        if 0 <= d[i] < 128:
            oha[bk[i], i] = 1.0
        else:
            oha[32, i] = 1.0
    blkoh = np.zeros((16, 4096), np.float32)
    for j in range(16):
        blkoh[j, j * 256:(j + 1) * 256] = 1.0
    return ident, ohm.astype(ml_dtypes.bfloat16), oha.astype(ml_dtypes.bfloat16), blkoh.astype(ml_dtypes.bfloat16)


def make_in_maps(x, c, ada_w, ada_b, norm1_g, norm2_g, w_in, attn_sinks, rel_bias, w_branch_a, w_branch_b, w_out,
                 w_mlp_in, w_mlp_out, final_g):
    f = lambda a: np.ascontiguousarray(np.asarray(a, dtype=np.float32))
    x, c = f(x), f(c)
    w_in0 = f(w_in)[0]
    qa = w_in0[:, 0:512]
    qa_perm = np.concatenate([np.concatenate([qa[:, j * 64:(j + 1) * 64], qa[:, (4 + j) * 64:(5 + j) * 64]], axis=1) for j in range(4)], axis=1)
    wqkv = np.concatenate([qa_perm, w_in0[:, 768:1280], w_in0[:, 1280:1792], w_in0[:, 512:640], w_in0[:, 1792:2304], w_in0[:, 640:768]], axis=1)
    wg = w_in0[:, 2304:4352]
    ident, ohm, oha, blkoh = _consts()
    shared = dict(
        ada_w=f(ada_w)[0], ada_b=f(ada_b)[0:1], g1=f(norm1_g)[0:1], g2=f(norm2_g)[0:1], gf=f(final_g).reshape(1, D),
        wqkv=np.ascontiguousarray(wqkv), wg=np.ascontiguousarray(wg), sinks=f(attn_sinks)[0:1], relb=f(rel_bias),
        wba=f(w_branch_a)[0], wbb=f(w_branch_b)[0], wout=f(w_out)[0], wmi=f(w_mlp_in)[0], wmo=f(w_mlp_out)[0],
        ident=ident, ohm=ohm, oha=oha, blkoh=blkoh)
    in_maps = []
    for core in range(8):
        b, par = core // 2, core % 2
        own_blocks = [2 * i + par for i in range(8)]
        xo = np.concatenate([x[b, blk * 256:(blk + 1) * 256] for blk in own_blocks], axis=0)
        oth = []
        for v in range(0, 16, 2):
            a = v - 1 if par == 0 else v
            oth.append(np.zeros((256, D), np.float32) if a < 0 else x[b, a * 256:(a + 1) * 256])
        xoth = np.concatenate(oth, axis=0)
        m = dict(shared)
        m.update(x_own=np.ascontiguousarray(xo), x_oth=np.ascontiguousarray(xoth),
                 ccol=np.ascontiguousarray(c[b].reshape(8, 128).T), flag=np.full((128, 1), 1.0 if par == 0 else 0.0, np.float32))
        in_maps.append(m)
    return in_maps


_PROG = {}


def kernel(**inputs):
    if 'nc' not in _PROG:
        _PROG['nc'], _PROG['info'] = build_program(None)
    nc = _PROG['nc']
    in_maps = make_in_maps(**inputs)
    res = run_bass_kernel_spmd(nc, in_maps, core_ids=list(range(8)))
    out = np.zeros((4, 4096, D), np.float32)
    for core in range(8):
        b, par = core // 2, core % 2
        o = res.results[core]["out"]
        for i in range(8):
            blk = 2 * i + par
            out[b, blk * 256:(blk + 1) * 256] = o[i * 256:(i + 1) * 256]
    return out
```

```python
import numpy as np
import ml_dtypes
import concourse.bass as bass
import concourse.mybir as mybir
from concourse.bass_utils import run_bass_kernel_spmd

F32 = mybir.dt.float32
BF16 = mybir.dt.bfloat16
AF = mybir.ActivationFunctionType
ALU = mybir.AluOpType
AX = mybir.AxisListType

NEG = -30000.0
SB_LIMIT = 212800
SB_BASE = 16512
D = 1024
NT_OWN = 16
LT_M = 3072
LW_M = 2560
LT_A = 512
LW_A = 384
DEBUG = None
PEB_MOD = 100000


def _isz(dt):
    return 4 if dt == F32 else 2


class Buf:
    def __init__(self, h, space, base, size, shape, dtype):
        self.h, self.space, self.base, self.size, self.shape, self.dtype = h, space, base, size, shape, dtype
        inner = 1
        for s in shape[2:]:
            inner *= s
        self.inner = inner

    def ap(self):
        return self.h.ap()

    def sl(self, i, n=1):
        e = _isz(self.dtype)
        return (self, i * self.inner * e, (i + n) * self.inner * e)

    def el(self, lo, hi):
        e = _isz(self.dtype)
        return (self, lo * e, hi * e)


def _norm_region(r):
    if isinstance(r, Buf):
        return (r.space, r.base, r.base + r.size)
    b, lo, hi = r
    return (b.space, b.base + lo, b.base + hi)


class Sched:
    def __init__(self, nc):
        self.nc = nc
        self.ops = []

    def add(self, eng, fn, reads=(), writes=(), key=None):
        rr = [_norm_region(r) for r in reads]
        ww = [_norm_region(w) for w in writes]
        if key is not None:
            ww.append(('key:' + key, 0, 1))
        self.ops.append((eng, fn, rr, ww, key))

    def emit(self):
        nc = self.nc
        ops = self.ops
        n = len(ops)
        handles = {'pe': nc.tensor, 'dve': nc.vector, 'act': nc.scalar, 'pool': nc.gpsimd, 'sp': nc.sync}
        writers = {}
        readers = {}
        deps = [None] * n
        needs = [False] * n
        for i, (eng, fn, rds, wrs, key) in enumerate(ops):
            d = set()
            for (sp, lo, hi) in rds:
                for w in writers.get(sp, ()):
                    if w[0] < hi and lo < w[1]:
                        d.add(w[2])
            for (sp, lo, hi) in wrs:
                for w in writers.get(sp, ()):
                    if w[0] < hi and lo < w[1]:
                        d.add(w[2])
                for r in readers.get(sp, ()):
                    if r[0] < hi and lo < r[1]:
                        d.add(r[2])
            d.discard(i)
            if eng == 'pe':
                d = {j for j in d if ops[j][0] != 'pe' or ops[j][4] is not None}
            deps[i] = d
            for j in d:
                needs[j] = True
            ek = eng if key is None else ('dma', key)
            for (sp, lo, hi) in rds:
                lst = readers.setdefault(sp, [])
                lst[:] = [r for r in lst if not (r[3] == ek and r[0] >= lo and r[1] <= hi)]
                lst.append([lo, hi, i, ek])
            for (sp, lo, hi) in wrs:
                lst = readers.setdefault(sp, [])
                lst[:] = [r for r in lst if not (r[0] >= lo and r[1] <= hi)]
                wl = writers.setdefault(sp, [])
                wl[:] = [w for w in wl if not (w[0] >= lo and w[1] <= hi)]
                wl.append([lo, hi, i])
        sem_eng = {e: nc.alloc_semaphore(f"se_{e}") for e in ('pe', 'dve', 'act', 'pool')}
        cnt_eng = {e: 0 for e in sem_eng}
        dma_sems = {}
        done = [None] * n
        seen = {}
        nwait = 0
        for i, (eng, fn, rds, wrs, key) in enumerate(ops):
            h = handles[eng]
            for dj in sorted(deps[i]):
                if done[dj] is None:
                    raise RuntimeError(f"dep {dj} of op {i} has no completion")
                sname, semobj, val = done[dj]
                if seen.get((eng, sname), 0) >= val:
                    continue
                h.wait_ge(semobj, val)
                nwait += 1
                seen[(eng, sname)] = val
            res = fn(h)
            if key is not None:
                insts = res if isinstance(res, list) else [res]
                if key not in dma_sems:
                    dma_sems[key] = [nc.alloc_semaphore(f"sd_{len(dma_sems)}"), 0]
                ent = dma_sems[key]
                for ins in insts:
                    ins.then_inc(ent[0], 16)
                    ent[1] += 16
                done[i] = (('dma', key), ent[0], ent[1])
            elif eng == 'sp':
                done[i] = None
            else:
                if needs[i]:
                    cnt_eng[eng] += 1
                    res.then_inc(sem_eng[eng], 1)
                    done[i] = (eng, sem_eng[eng], cnt_eng[eng])
                else:
                    done[i] = (eng, sem_eng[eng], cnt_eng[eng] + 0) if False else None
        return dict(nops=n, nwait=nwait, cnt=cnt_eng, ndma=len(dma_sems))


def t5_bucket_np(d):
    d = np.asarray(d)
    n = np.maximum(d, 0)
    nf = np.maximum(n, 1).astype(np.float32)
    large = 16 + (np.log(nf / np.float32(16)) / np.float32(np.log(2048 / 16)) * np.float32(16)).astype(np.int32)
    large = np.minimum(large, 31)
    return np.where(n < 16, n, large)


def build_program(debug=None):
    nc = bass.Bass("TRN2", target_bir_lowering=False)
    S = Sched(nc)
    cnt = [0]

    def dram(name, shape, dt, kind):
        h = nc.dram_tensor(name, shape, dt, kind=kind)
        sz = 1
        for s in shape:
            sz *= s
        return Buf(h, 'dram:' + name, 0, sz * _isz(dt), [1] + list(shape), dt)

    def sb(name, shape, dt, off):
        assert off % 32 == 0, (name, off)
        cnt[0] += 1
        sz = _isz(dt)
        for s in shape[1:]:
            sz *= s
        assert off + sz <= SB_LIMIT, (name, off, sz)
        h = nc.alloc_sbuf_tensor_at(f"{name}_{cnt[0]}", list(shape), dt, offset=SB_BASE + off)
        return Buf(h, 'sb', off, sz, list(shape), dt)

    I = 'ExternalInput'
    x_own = dram("x_own", [2048, D], F32, I)
    x_oth = dram("x_oth", [2048, D], F32, I)
    ccol = dram("ccol", [128, 8], F32, I)
    flag_d = dram("flag", [128, 1], F32, I)
    ada_w = dram("ada_w", [D, 6 * D], F32, I)
    ada_b = dram("ada_b", [1, 6 * D], F32, I)
    g1_d = dram("g1", [1, D], F32, I)
    g2_d = dram("g2", [1, D], F32, I)
    gf_d = dram("gf", [1, D], F32, I)
    wqkv_d = dram("wqkv", [D, 2304], F32, I)
    wg_d = dram("wg", [D, 2048], F32, I)
    sinks_d = dram("sinks", [1, 8], F32, I)
    relb_d = dram("relb", [32, 16], F32, I)
    wba_d = dram("wba", [512, D], F32, I)
    wbb_d = dram("wbb", [512, D], F32, I)
    wout_d = dram("wout", [D, D], F32, I)
    wmi_d = dram("wmi", [D, 4 * D], F32, I)
    wmo_d = dram("wmo", [4 * D, D], F32, I)
    ident_d = dram("ident", [128, 128], BF16, I)
    ohm_d = dram("ohm", [33, LT_M], BF16, I)
    oha_d = dram("oha", [33, LT_A], BF16, I)
    blkoh_d = dram("blkoh", [16, 4096], BF16, I)
    out_d = dram("out", [2048, D], F32, 'ExternalOutput')
    mod_d = dram("mod_scr", [1, 6 * D], F32, 'Internal')
    scrm_d = dram("scr_m", [8, 128, LT_M], BF16, 'Internal')
    scra_d = dram("scr_a", [8, 128, LT_A], BF16, 'Internal')
    dbg_d = None
    if debug:
        dbg_d = dram("dbg", [128, 8192], F32, 'ExternalOutput')

    PS = []
    for b in range(8):
        h = nc.alloc_psum_tensor(f"psb{b}", [128, 512], F32)
        PS.append(Buf(h, 'ps', b * 2048, 2048, [128, 512], F32))

    def ps_bf(b):
        return PS[b].ap().bitcast(BF16)

    def dma(eng, key, out_ap, in_ap, reads, writes):
        S.add(eng, lambda h, o=out_ap, i=in_ap: h.dma_start(out=o, in_=i), reads, writes, key=key)

    def bcast_rows(d_ap_row, nparts=128):
        t = d_ap_row
        return bass.AP(t.tensor, t.offset, [[0, nparts]] + [list(x) for x in t.ap[1:]])

    ident = sb("ident", [128, 128], BF16, 0)
    onesf = sb("onesf", [128, 128], F32, 256)
    flag = sb("flag", [128, 4], F32, 768)
    esink = sb("esink", [128, 8], F32, 800)
    rb31 = sb("rb31", [128, 16], F32, 832)
    cs = sb("cs", [128, 8], F32, 896)
    epsb = sb("epsb", [128, 1], F32, 928)
    ccs = sb("ccs", [128, 8], F32, 960)
    pm = sb("pm", [128, 16, 16], F32, 1024)
    basem = sb("basem", [128, 16, 16], F32, 2048)
    zeros = sb("zeros", [128, 128], F32, 3072)
    stat = sb("stat", [128, 8], F32, 3584)

    dma('sp', 'c_ident', ident.ap(), ident_d.ap(), [ident_d], [ident])
    dma('sp', 'c_flag', flag.ap()[:, 0:1], flag_d.ap(), [flag_d], [flag])
    dma('sp', 'c_cc', ccs.ap(), ccol.ap(), [ccol], [ccs])
    dma('sp', 'c_sink', esink.ap(), bcast_rows(sinks_d.ap()), [sinks_d], [esink])
    dma('sp', 'c_rb31', rb31.ap(), bcast_rows(relb_d.ap()[31:32, :]), [relb_d], [rb31])
    S.add('dve', lambda h: h.memset(onesf.ap(), 1.0), [], [onesf])
    S.add('dve', lambda h: h.memset(zeros.ap(), 0.0), [], [zeros])
    S.add('dve', lambda h: h.memset(epsb.ap(), 1e-6), [], [epsb])
    S.add('dve', lambda h: h.tensor_scalar(out=flag.ap()[:, 1:2], in0=flag.ap()[:, 0:1], scalar1=NEG, scalar2=None,
                                           op0=ALU.mult), [flag], [flag])
    S.add('dve', lambda h: h.tensor_scalar(out=flag.ap()[:, 2:3], in0=flag.ap()[:, 0:1], scalar1=-2e30, scalar2=None,
                                           op0=ALU.mult), [flag], [flag])
    S.add('act', lambda h: h.activation(out=esink.ap(), in_=esink.ap(), func=AF.Exp), [esink], [esink])
    S.add('act', lambda h: h.activation(out=cs.ap(), in_=ccs.ap(), func=AF.Silu), [ccs], [cs])
    S.add('pool', lambda h: h.memset(pm.ap(), -2e30), [], [pm])
    S.add('pool', lambda h: h.memset(basem.ap(), NEG), [], [basem])
    for s in range(8):
        S.add('pool', lambda h, s=s: h.memset(pm.ap()[:, 2 * s:2 * s + 2, 0:2 * s + 1], 0.0), [], [pm])
        S.add('pool', lambda h, s=s: h.memset(basem.ap()[:, 2 * s:2 * s + 2, 2 * s + 1:2 * s + 2], 0.0), [], [basem])
    S.add('dve', lambda h: h.tensor_scalar(out=pm.ap()[:, :, 0:1], in0=pm.ap()[:, :, 0:1], scalar1=flag.ap()[:, 2:3],
                                           scalar2=None, op0=ALU.add), [pm, flag], [pm])

    O_P0 = 4096
    rbs = sb("rbs", [33, 16], F32, O_P0)
    rbB = sb("rbB", [33, 16, 128], BF16, O_P0 + 64)
    ohm = sb("ohm", [33, LT_M], BF16, O_P0 + 4160)
    oha = sb("oha", [33, LT_A], BF16, O_P0 + 10304)
    tst = [sb("tst", [128, LT_M], BF16, O_P0 + 11328 + i * 6144) for i in range(2)]
    mp1 = [sb("mp1", [128, 2048], F32, o_) for o_ in (70144, 78336, 103040, 111232)]

    def mod_pass(c0, ncols, bufs, acc, keyp, bank_fn, eng='dve', dq='sp', rowbuf=None):
        nb = len(bufs)

        def load(k):
            if k < 8:
                dma(dq, f'{keyp}{k % nb}', bufs[k % nb].ap()[:, 0:ncols], ada_w.ap()[k * 128:(k + 1) * 128, c0:c0 + ncols],
                    [ada_w], [bufs[k % nb]])
            else:
                dma(dq, f'{keyp}{k % nb}', bufs[k % nb].ap()[0:1, 0:ncols], ada_b.ap()[:, c0:c0 + ncols], [ada_b], [bufs[k % nb]])

        def accf(k):
            bw = bufs[k % nb]
            if k == 0:
                S.add(eng, lambda h: h.tensor_scalar(out=acc.ap()[:, 0:ncols], in0=bw.ap()[:, 0:ncols], scalar1=cs.ap()[:, 0:1], scalar2=0.0,
                                                     op0=ALU.mult, op1=ALU.add), [bw, cs], [acc])
            elif k < 8 and eng == 'dve':
                S.add('dve', lambda h: h.scalar_tensor_tensor(out=acc.ap()[:, 0:ncols], in0=bw.ap()[:, 0:ncols], scalar=cs.ap()[:, k:k + 1],
                                                              in1=acc.ap()[:, 0:ncols], op0=ALU.mult, op1=ALU.add), [bw, cs, acc], [acc])
            elif k < 8:
                S.add(eng, lambda h: h.tensor_scalar(out=bw.ap()[:, 0:ncols], in0=bw.ap()[:, 0:ncols], scalar1=cs.ap()[:, k:k + 1], scalar2=0.0,
                                                     op0=ALU.mult, op1=ALU.add), [bw, cs], [bw])
                S.add(eng, lambda h: h.tensor_tensor(out=acc.ap()[:, 0:ncols], in0=acc.ap()[:, 0:ncols], in1=bw.ap()[:, 0:ncols], op=ALU.add),
                      [bw, acc], [acc])
            else:
                S.add(eng, lambda h: h.tensor_tensor(out=acc.ap()[0:1, 0:ncols], in0=acc.ap()[0:1, 0:ncols], in1=bw.ap()[0:1, 0:ncols], op=ALU.add),
                      [bw, acc], [acc])

        def fin():
            ob_ = acc if rowbuf is None else rowbuf
            for p0 in range(0, ncols, 512):
                pb = bank_fn()
                S.add('pe', lambda h, p0=p0, pb=pb: h.matmul(pb.ap()[0:1, :], lhsT=onesf.ap()[:, 0:1], rhs=acc.ap()[:, p0:p0 + 512], start=True, stop=True),
                      [onesf, acc], [pb])
                S.add('dve', lambda h, p0=p0, pb=pb: h.tensor_copy(out=ob_.ap()[0:1, p0:p0 + 512], in_=pb.ap()[0:1, :]), [pb],
                      [ob_.el(p0, p0 + 512) if rowbuf is not None else ob_])
            dma('sp', f'{keyp}st', mod_d.ap()[:, c0:c0 + ncols], ob_.ap()[0:1, 0:ncols], [ob_], [mod_d.el(c0, c0 + ncols)])
        return load, accf, fin

    p0ctr = [0]

    def p0_bank():
        p0ctr[0] += 1
        return PS[p0ctr[0] % 4]

    deferred_fin = []
    modrow = sb("modrow", [1, 2048], F32, 27712)
    ld_, ac_, fin_ = mod_pass(0, 2048, mp1[0:3], mp1[3], 'mp0', p0_bank, eng='dve', rowbuf=modrow)
    for k in range(3):
        ld_(k)
    for k in range(9):
        ac_(k)
        if k + 3 < 9:
            ld_(k + 3)
    fin_()

    def emit_t5():
        dma('sp', 'c_rbs', rbs.ap()[0:32, :], relb_d.ap(), [relb_d], [rbs])
        dma('sp', 'c_ohm', ohm.ap(), ohm_d.ap(), [ohm_d], [ohm])
        dma('sp', 'c_oha', oha.ap(), oha_d.ap(), [oha_d], [oha])
        S.add('dve', lambda h: h.memset(rbB.ap(), NEG), [], [rbB])
        S.add('dve', lambda h: h.tensor_copy(out=rbB.ap()[0:32, :, :],
                                             in_=rbs.ap()[0:32, :].unsqueeze(2).to_broadcast([32, 16, 128])), [rbs, rbB], [rbB])
        tcount = 0
        for hh in range(16):
            is_a = hh < 8
            oh = oha if is_a else ohm
            LT = LT_A if is_a else LT_M
            st = tst[hh % 2]
            nch = LT // 512
            for ch in range(nch):
                pb = PS[2 + (tcount % 2)]
                tcount += 1
                S.add('pe', lambda h, pb=pb, hh=hh, oh=oh, ch=ch: h.matmul(
                    pb.ap(), lhsT=rbB.ap()[0:33, hh, :], rhs=oh.ap()[0:33, ch * 512:(ch + 1) * 512], start=True, stop=True),
                    [rbB, oh], [pb])
                S.add('act', lambda h, pb=pb, st=st, ch=ch: h.copy(out=st.ap()[:, ch * 512:(ch + 1) * 512], in_=pb.ap()),
                      [pb], [st.el(ch * 512, (ch + 1) * 512)])
            if is_a:
                dma('sp', f'tst{hh % 2}', scra_d.ap()[hh], st.ap()[:, 0:LT_A], [st],
                    [scra_d.el(hh * 128 * LT_A, (hh + 1) * 128 * LT_A)])
            else:
                dma('sp', f'tst{hh % 2}', scrm_d.ap()[hh - 8], st.ap(), [st],
                    [scrm_d.el((hh - 8) * 128 * LT_M, (hh - 7) * 128 * LT_M)])


    O_K = 4096
    Kst = sb("Kst", [128, 4, 4096], BF16, O_K)
    O_V = O_K + 32768
    Vb = sb("Vb", [128, 32, 8, 65], BF16, O_V)
    O_Q = O_V + 33280
    Qst = sb("Qst", [128, 4, 2048], BF16, O_Q)
    O_SWA = O_Q + 16384
    kaT = sb("kaT", [128, 4096], BF16, O_SWA)
    va = sb("va", [128, 32, 2, 65], BF16, O_SWA + 8192)
    qaT = sb("qaT", [128, 4, 2048], BF16, O_SWA + 16512)
    O_W1 = O_SWA + 32896
    wkv = sb("wkv", [128, 8, 1280], BF16, O_W1)
    wq = sb("wq", [128, 8, 1024], BF16, O_W1 + 20480)
    O_F = O_W1 + 36864
    G1bc = sb("G1bc", [128, D], F32, O_F)
    SH1bc = sb("SH1bc", [128, D], F32, O_F + 4096)
    xt = [sb("xt", [128, D], F32, O_F + 8192 + i * 4096) for i in range(2)]
    hb = [sb("hb", [128, D], BF16, O_F + 16384 + i * 2048) for i in range(2)]
    hT = [sb("hT", [128, 8, 512], BF16, O_F + 20480 + i * 8192) for i in range(2)]
    g1bc = sb("g1bc", [128, D], F32, O_F + 36896)
    xt.append(sb("xt", [128, D], F32, O_F + 36896 + 4096))
    hb.append(sb("hb", [128, D], BF16, O_F + 36896))

    def load_mod_bc(dst, idx, key):
        dma('sp', key, dst.ap(), bcast_rows(mod_d.ap()[:, idx * D:(idx + 1) * D]), [mod_d.el(idx * D, (idx + 1) * D)], [dst])

    dma('sp', 'c_g1b', g1bc.ap(), bcast_rows(g1_d.ap()), [g1_d], [g1bc])
    for pc in range(4):
        bb_ = PS[4 + pc]
        S.add('pe', lambda h, pc=pc, bb_=bb_: h.matmul(bb_.ap(), lhsT=onesf.ap()[0:1, 0:128], rhs=modrow.ap()[0:1, pc * 512:(pc + 1) * 512],
                                                       start=True, stop=True), [onesf, modrow.el(pc * 512, (pc + 1) * 512)], [bb_])
        if pc < 2:
            S.add('act', lambda h, pc=pc, bb_=bb_: h.copy(out=SH1bc.ap()[:, pc * 512:(pc + 1) * 512], in_=bb_.ap()), [bb_],
                  [SH1bc.el(pc * 512, (pc + 1) * 512)])
        else:
            c0_ = (pc - 2) * 512
            S.add('dve', lambda h, c0_=c0_, bb_=bb_: h.scalar_tensor_tensor(out=G1bc.ap()[:, c0_:c0_ + 512], in0=bb_.ap(), scalar=1.0,
                                                                            in1=g1bc.ap()[:, c0_:c0_ + 512], op0=ALU.add, op1=ALU.mult),
                  [bb_, g1bc], [G1bc.el(c0_, c0_ + 512)])

    def load_w(dst, d_view_pkn, ncols, key, src_buf, col0=0, kchunks=8, dst_k0=0):
        step = 1024 if ncols % 1024 == 0 else (1152 if ncols % 1152 == 0 else ncols)
        for k in range(kchunks):
            def f(h, k=k):
                res = []
                for c0 in range(0, ncols, step):
                    res.append(h.dma_start(out=dst.ap()[:, dst_k0 + k, col0 + c0:col0 + c0 + step],
                                           in_=d_view_pkn[:, k, c0:c0 + step]))
                return res
            S.add('pool', f, [src_buf], [dst.sl(dst_k0 + k)], key=f'{key}_{k % 4}')


    wqkv_v = wqkv_d.ap().rearrange("(k p) n -> p k n", p=128)
    load_w(wkv, wqkv_v[:, :, 1024:2304], 1280, 'w_kv', wqkv_d)
    load_w(wq, wqkv_v[:, :, 0:1024], 1024, 'w_kv', wqkv_d)

    S.add('pool', lambda h: h.memset(Vb.ap()[:, :, :, 64:65], 1.0), [], [Vb])
    S.add('pool', lambda h: h.memset(va.ap()[:, :, :, 64:65], 1.0), [], [va])

    stat2 = [stat, sb("statb", [128, 8], F32, 3616)]
    fe_ctr = [0]

    def frontend(src_ap, src_reads, xbuf, hbuf, hTbuf, tcol, Gbc, SHbc, psb, xkey=None):
        st_ = stat2[fe_ctr[0] % 2]
        fe_ctr[0] += 1

        def elem():
            dma('sp', xkey, xbuf.ap(), src_ap, src_reads, [xbuf])
            S.add('act', lambda h: h.activation(out=hbuf.ap(), in_=xbuf.ap(), func=AF.Square, accum_out=st_.ap()[:, 0:1]),
                  [xbuf], [hbuf, st_])
            S.add('act', lambda h: h.activation(out=st_.ap()[:, 1:2], in_=st_.ap()[:, 0:1], func=AF.Sqrt,
                                                bias=epsb.ap()[:, 0:1], scale=1.0 / D), [st_, epsb], [st_])
            S.add('dve', lambda h: h.reciprocal(out=st_.ap()[:, 2:3], in_=st_.ap()[:, 1:2]), [st_], [st_])
            S.add('dve', lambda h: h.scalar_tensor_tensor(out=xbuf.ap(), in0=xbuf.ap(), scalar=st_.ap()[:, 2:3], in1=Gbc.ap(),
                                                          op0=ALU.mult, op1=ALU.mult), [xbuf, st_, Gbc], [xbuf])
            S.add('dve', lambda h: h.tensor_tensor(out=hbuf.ap(), in0=xbuf.ap(), in1=SHbc.ap(), op=ALU.add),
                  [xbuf, SHbc], [hbuf])

        def tr_():
            pv = ps_bf(psb).rearrange("p (c t) -> p c t", c=8)

            def tr(h):
                r = None
                for c in range(8):
                    r = h.transpose(out=pv[:, c, :], in_=hbuf.ap()[:, c * 128:(c + 1) * 128], identity=ident.ap())
                return r
            S.add('pe', tr, [hbuf, ident], [PS[psb]])
            S.add('act', lambda h: h.copy(out=hTbuf.ap()[:, :, tcol * 128:(tcol + 1) * 128], in_=pv), [PS[psb]], [hTbuf])
        return elem, tr_

    def interleave(units, extras):
        n = len(units)
        sched = {}
        for pos, f in extras:
            sched.setdefault(max(0, min(n - 1, pos)), []).append(f)
        for u in range(n):
            for f in sched.get(u, []):
                f()
            units[u]()

    evac_rr = [0]

    def evac(out_ap, in_ap, reads, writes, scale=None):
        e = 'act' if evac_rr[0] % 2 == 0 else 'dve'
        evac_rr[0] += 1
        if e == 'act':
            if scale is None:
                S.add('act', lambda h: h.copy(out=out_ap, in_=in_ap), reads, writes)
            else:
                S.add('act', lambda h: h.activation(out=out_ap, in_=in_ap, func=AF.Identity, scale=scale), reads, writes)
        else:
            if scale is None:
                S.add('dve', lambda h: h.tensor_copy(out=out_ap, in_=in_ap), reads, writes)
            else:
                S.add('dve', lambda h: h.tensor_scalar(out=out_ap, in0=in_ap, scalar1=scale, scalar2=None, op0=ALU.mult),
                      reads, writes)

    proj_rr = [0]

    def next_bank():
        b = 2 + (proj_rr[0] % 5)
        proj_rr[0] += 1
        return b

    def fm_proj(hTbuf, wbuf, wcol0, nk, out_ap, writes, scale=None, rhs_fn=None):
        b = next_bank()

        def mm(h):
            r = None
            for k in range(nk):
                rhs = hTbuf.ap()[:, k, :] if rhs_fn is None else rhs_fn(k)
                r = h.matmul(PS[b].ap(), lhsT=wbuf.ap()[:, k, wcol0:wcol0 + 128], rhs=rhs, start=(k == 0), stop=(k == nk - 1))
            return r
        S.add('pe', mm, [hTbuf, wbuf], [PS[b]])
        evac(out_ap, PS[b].ap(), [PS[b]], writes, scale)

    def phase1_frontend(g, pos):
        own = g < 4
        src = x_own if own else x_oth
        gg = g if own else g - 4
        res = []
        for t in range(4):
            tile_i = gg * 4 + t
            idx = g * 4 + t
            res.append(frontend(src.ap()[tile_i * 128:(tile_i + 1) * 128, :], [src], xt[idx % 3], hb[idx % 3], hT[pos % 2], t,
                                G1bc, SH1bc, idx % 2, xkey=f'xt{idx % 3}'))
        return res

    def phase1_proj(g, pos):
        units = []
        own = g < 4
        gg = g if own else g - 4
        hTb = hT[pos % 2]
        vb0 = (4 * gg + 1) if own else (4 * gg)
        kcol = vb0 * 256
        for c in range(4):
            dst = Kst.ap()[:, c, :]
            dst = bass.AP(dst.tensor, dst.offset + kcol, [list(dst.ap[0]), [512, 2], [1, 256]])
            units.append(lambda c=c, dst=dst: fm_proj(hTb, wkv, c * 128, 8, dst, [Kst.sl(c)]))
        dst = kaT.ap()
        dst = bass.AP(dst.tensor, dst.offset + kcol, [list(dst.ap[0]), [512, 2], [1, 256]])
        units.append(lambda dst=dst: fm_proj(hTb, wkv, 512, 8, dst, [kaT]))
        if own:
            for c in range(4):
                units.append(lambda c=c: fm_proj(hTb, wq, c * 128, 8, qaT.ap()[:, c, gg * 512:(gg + 1) * 512], [qaT.sl(c)], scale=0.125))
            for c in range(4):
                units.append(lambda c=c: fm_proj(hTb, wq, 512 + c * 128, 8, Qst.ap()[:, c, gg * 512:(gg + 1) * 512], [Qst.sl(c)], scale=0.125))
        for t in range(4):
            vt = (vb0 + (t // 2) * 2) * 2 + (t % 2)

            def uv(t=t, vt=vt):
                b = next_bank()

                def mm(h):
                    r = None
                    for k in range(8):
                        r = h.matmul(PS[b].ap(), lhsT=hTb.ap()[:, k, t * 128:(t + 1) * 128], rhs=wkv.ap()[:, k, 640:1152],
                                     start=(k == 0), stop=(k == 7))
                    return r
                S.add('pe', mm, [hTb, wkv], [PS[b]])
                evac(Vb.ap()[:, vt, :, 0:64], PS[b].ap().rearrange("p (h d) -> p h d", h=8), [PS[b]], [Vb.sl(vt)])
            units.append(uv)

            def uv2(t=t, vt=vt):
                b2 = next_bank()

                def mm2(h):
                    r = None
                    for k in range(8):
                        r = h.matmul(PS[b2].ap()[:, 0:128], lhsT=hTb.ap()[:, k, t * 128:(t + 1) * 128], rhs=wkv.ap()[:, k, 1152:1280],
                                     start=(k == 0), stop=(k == 7))
                    return r
                S.add('pe', mm2, [hTb, wkv], [PS[b2]])
                evac(va.ap()[:, vt, :, 0:64], PS[b2].ap()[:, 0:128].rearrange("p (h d) -> p h d", h=2), [PS[b2]], [va.sl(vt)])
            units.append(uv2)
        return units

    order = [4, 5, 6, 7, 0, 1, 2, 3]
    fe0 = phase1_frontend(order[0], 0)
    for e_, t_ in fe0[0:3]:
        e_()
    fe0[0][1]()
    fe0[3][0]()
    fe0[1][1]()
    fe0[2][1]()
    fe0[3][1]()
    emit_t5()
    for i, g in enumerate(order):
        units = phase1_proj(g, i)
        n = len(units)
        extras = []
        if i + 1 < len(order):
            fes = phase1_frontend(order[i + 1], i + 1)
            for t, (e_, t_) in enumerate(fes):
                p0 = (t * n) // 4
                extras.append((p0, e_))
                extras.append((p0 + 5, t_))
        interleave(units, extras)

    def dump(ap, ncols, parts=128, col0=0):
        stg = sb("dbgstg", [128, 2048], F32, SB_LIMIT - 8192 - 32)
        for c0 in range(0, ncols, 2048):
            w = min(2048, ncols - c0)
            S.add('dve', lambda h, c0=c0, w=w: h.tensor_copy(out=stg.ap()[0:parts, 0:w], in_=ap[:, c0:c0 + w]),
                  [Buf(None, 'sb', 0, SB_LIMIT, [1], F32)], [stg])
            dma('sp', 'dbg', dbg_d.ap()[0:parts, col0 + c0:col0 + c0 + w], stg.ap()[0:parts, 0:w], [stg], [dbg_d])

    def finish():
        S.add('sp', lambda h: None, [out_d] + ([dbg_d] if dbg_d is not None else []), [])
        info = S.emit()
        return nc, info

    if debug == 'p1':
        dump(Qst.ap()[:, 0, :], 2048, col0=0)
        dump(Kst.ap()[:, 0, :], 4096, col0=2048)
        dump(Vb.ap()[:, 3, :, :].rearrange("p h d -> p (h d)"), 520, col0=6144)
        dump(qaT.ap()[:, 1, 0:512], 512, col0=6144 + 520)
        dump(kaT.ap()[:, 0:512], 512, col0=6144 + 1032)
        return finish()

    Oa = sb("Oa", [128, 16, 512], BF16, O_W1)
    Ob = sb("Ob", [128, 16, 512], BF16, O_W1 + 16384)
    O_X = O_W1 + 32768
    Wa = sb("Wa", [128, 8, LW_A], BF16, O_X)
    Wa2 = sb("Wa2", [128, 8, 128], BF16, O_X + 6144)
    Sp = [sb("Sp", [128, 512], F32, O_X + 8192 + i * 2048) for i in range(3)]
    Pt = [sb("Pt", [128, 512], BF16, O_X + 14336 + i * 1024) for i in range(3)]
    den = sb("den", [128, 16], F32, O_X + 17408)
    gm = sb("gm", [128, 16, 16], F32, O_X + 17472)
    mx = sb("mx", [128, 16, 8], F32, O_X + 18496)
    selb = sb("selb", [128, 16, 16], F32, O_X + 19008)
    Mtok = sb("Mtok", [128, 16, 16], BF16, O_X + 20032)
    ksum = sb("ksum", [128, 4, 16], F32, O_X + 20544)
    khi = sb("khi", [128, 4, 16], BF16, O_X + 20800)
    klo = sb("klo", [128, 4, 16], BF16, O_X + 20928)
    ktmp = sb("ktmp", [128, 4, 16], F32, O_X + 21056)
    Wm = [sb("Wm", [128, LW_M], BF16, O_X + 21312 + i * 5120) for i in range(2)]
    O_X2 = O_X + 31552
    SpA = [sb("SpA", [128, 512], F32, O_X2 + i * 2048) for i in range(4)]
    PtA = [sb("PtA", [128, 512], BF16, O_X2 + 8192 + i * 1024) for i in range(4)]
    denA = [sb("denA", [128, 16], F32, O_X2 + 12288 + i * 64) for i in range(4)]
    Kaug = [sb("Kaug", [80, 4096], BF16, O_SWA + i * 8192) for i in range(2)]
    Qaug = [sb("Qaug", [80, 2048], BF16, O_SWA + 16384 + i * 4096) for i in range(2)]

    for hh in range(8):
        src = bass.AP(scra_d.h, hh * 128 * LT_A + 127, [[LT_A - 1, 128], [1, LW_A]])
        dma('sp', f'c_wa{hh % 4}', Wa.ap()[:, hh, :], src, [scra_d], [Wa.sl(hh)])
    S.add('dve', lambda h: h.tensor_scalar(out=Wa2.ap(), in0=Wa.ap()[:, :, 256:384], scalar1=flag.ap()[:, 1:2], scalar2=None,
                                           op0=ALU.add), [Wa, flag], [Wa2])

    swa_its = [(qt, g) for qt in range(16) for g in range(2)]

    def swa_bufs(it):
        sbank = [PS[0], PS[1]] if it % 2 == 0 else [PS[4], PS[5]]
        obank = PS[2 + (it % 2)]
        sp_ = [SpA[2 * (it % 2)], SpA[2 * (it % 2) + 1]]
        pt_ = [PtA[2 * (it % 2)], PtA[2 * (it % 2) + 1]]
        return sbank, obank, sp_, pt_, denA[it % 2]

    def swa_front(it):
        qt, g = swa_its[it]
        s = qt // 2
        vt = (2 * s + 1) * 2 + (qt % 2)
        sbank, obank, sp_, pt_, dnb = swa_bufs(it)
        for j, kt in enumerate((vt - 1, vt)):
            S.add('pe', lambda h, j=j, kt=kt: h.matmul(
                sbank[j].ap(), lhsT=kaT.ap()[64 * g:64 * g + 64, kt * 128:(kt + 1) * 128],
                rhs=qaT.ap()[64 * g:64 * g + 64, :, qt * 128:(qt + 1) * 128], start=True, stop=True),
                [kaT, qaT], [sbank[j]])
            if j == 0:
                tab = Wa2.ap()[:, 4 * g:4 * g + 4, :] if qt == 0 else Wa.ap()[:, 4 * g:4 * g + 4, 256:384]
            else:
                tab = Wa.ap()[:, 4 * g:4 * g + 4, 128:256]
            S.add('dve', lambda h, j=j, tab=tab: h.tensor_tensor(
                out=sp_[j].ap().rearrange("p (a q) -> p a q", a=4), in0=sbank[j].ap().rearrange("p (a q) -> p a q", a=4),
                in1=tab, op=ALU.add), [sbank[j], Wa, Wa2], [sp_[j]])
            S.add('act', lambda h, j=j: h.activation(out=pt_[j].ap(), in_=sp_[j].ap(), func=AF.Exp), [sp_[j]], [pt_[j]])

    def swa_back(it):
        qt, g = swa_its[it]
        s = qt // 2
        vt = (2 * s + 1) * 2 + (qt % 2)
        sbank, obank, sp_, pt_, dnb = swa_bufs(it)
        ov = obank.ap()[:, 0:260].rearrange("p (a d) -> p a d", a=4)

        def pv(h):
            r = None
            for a in range(4):
                for j, kt in enumerate((vt - 1, vt)):
                    r = h.matmul(ov[:, a, :], lhsT=pt_[j].ap()[:, a * 128:(a + 1) * 128], rhs=va.ap()[:, kt, g, :],
                                 start=(j == 0), stop=(j == 1))
            return r
        S.add('pe', pv, [pt_[0], pt_[1], va], [obank])
        S.add('dve', lambda h: h.tensor_tensor(out=dnb.ap()[:, 0:4], in0=ov[:, :, 64], in1=esink.ap()[:, 4 * g:4 * g + 4],
                                               op=ALU.add), [obank, esink], [dnb])
        S.add('dve', lambda h: h.reciprocal(out=dnb.ap()[:, 4:8], in_=dnb.ap()[:, 0:4]), [dnb], [dnb])
        S.add('dve', lambda h: h.tensor_tensor(
            out=Oa.ap()[:, qt, 256 * g:256 * g + 256].rearrange("p (a d) -> p a d", a=4), in0=ov[:, :, 0:64],
            in1=dnb.ap()[:, 4:8].unsqueeze(2).to_broadcast([128, 4, 64]), op=ALU.mult), [obank, dnb], [Oa.sl(qt)])


    S.add('dve', lambda h: h.tensor_reduce(out=ksum.ap().rearrange("p c b -> p (c b)"),
                                           in_=Kst.ap().rearrange("p c (b k) -> p (c b) k", k=256), op=ALU.add, axis=AX.X),
          [Kst], [ksum])
    S.add('dve', lambda h: h.tensor_copy(out=khi.ap(), in_=ksum.ap()), [ksum], [khi])
    S.add('dve', lambda h: h.tensor_tensor(out=ktmp.ap(), in0=ksum.ap(), in1=khi.ap(), op=ALU.subtract), [ksum, khi], [ktmp])
    S.add('dve', lambda h: h.tensor_copy(out=klo.ap(), in_=ktmp.ap()), [ktmp], [klo])

    def moba_head_prep(hd):
        c, half = hd // 2, hd % 2
        pr = slice(64 * half, 64 * half + 64)
        ka_, qa_, wm_ = Kaug[hd % 2], Qaug[hd % 2], Wm[hd % 2]
        pbk = {}

        def stA2():
            ce = 'dve'
            S.add(ce, lambda h: h.tensor_copy(out=ka_.ap()[0:64, :], in_=Kst.ap()[pr, c, :]), [Kst], [ka_])
            S.add(ce, lambda h: h.tensor_copy(out=qa_.ap()[0:64, :], in_=Qst.ap()[pr, c, :]), [Qst], [qa_])

        def stA():
            stA2()
            stA1()

        def stA1():
            gb_ = sbank_next()
            gv = gb_.ap()[:, 0:256].rearrange("p (t b) -> p t b", t=16)
            src = bass.AP(scrm_d.h, hd * 128 * LT_M + 127, [[LT_M - 1, 128], [1, LW_M]])
            dma('sp', f'wm{hd % 2}', wm_.ap(), src, [scrm_d], [wm_])

            def gmm(h):
                r = None
                for qt in range(16):
                    r = h.matmul(gv[:, qt, :], lhsT=Qst.ap()[pr, c, qt * 128:(qt + 1) * 128], rhs=khi.ap()[pr, c, :], start=True, stop=False)
                    r = h.matmul(gv[:, qt, :], lhsT=Qst.ap()[pr, c, qt * 128:(qt + 1) * 128], rhs=klo.ap()[pr, c, :], start=False, stop=True)
                return r
            S.add('pe', gmm, [Qst, khi, klo], [gb_])
            S.add('dve', lambda h: h.tensor_tensor(out=gm.ap(), in0=gv, in1=pm.ap(), op=ALU.add), [gb_, pm], [gm])
            for qt in range(16):
                S.add('dve', lambda h, qt=qt: h.max(out=mx.ap()[:, qt, :], in_=gm.ap()[:, qt, :]), [gm], [mx])
            S.add('dve', lambda h: h.tensor_scalar(out=mx.ap()[:, :, 2:3], in0=mx.ap()[:, :, 2:3], scalar1=-1e30, scalar2=None,
                                                   op0=ALU.max), [mx], [mx])
            S.add('dve', lambda h: h.tensor_tensor(out=selb.ap(), in0=gm.ap(), in1=mx.ap()[:, :, 2:3].to_broadcast([128, 16, 16]),
                                                   op=ALU.is_ge), [gm, mx], [selb])
            S.add('dve', lambda h: h.scalar_tensor_tensor(out=Mtok.ap(), in0=selb.ap(), scalar=-NEG, in1=basem.ap(),
                                                          op0=ALU.mult, op1=ALU.add), [selb, basem], [Mtok])

        def stB(half2):
            tb_ = sbank_next()
            pbk[half2] = tb_
            pvv = tb_.ap().bitcast(BF16)

            def tr(h):
                r = None
                for t8 in range(8):
                    qt = half2 * 8 + t8
                    r = h.transpose(out=pvv[0:16, t8 * 128:(t8 + 1) * 128], in_=Mtok.ap()[:, qt, :], identity=ident.ap())
                return r
            S.add('pe', tr, [Mtok, ident], [tb_])

        def stC(half2):
            tb_ = pbk[half2]
            pvv = tb_.ap().bitcast(BF16)
            S.add('act', lambda h: h.copy(out=qa_.ap()[64:80, half2 * 1024:(half2 + 1) * 1024], in_=pvv[0:16, 0:1024]), [tb_], [qa_])
        return [stA, lambda: (stB(0), stC(0)), lambda: (stB(1), stC(1)), stA1, stA2]

    iters = []
    for hd in range(8):
        for s in range(8):
            nblk = 2 * s + 2
            for j in range(nblk):
                iters.append((hd, s, j, j == 0, j == nblk - 1))

    def it_far(itx):
        hd, s, j, first, last = iters[itx]
        return (2 * s + 1 - j) * 256 - 128 >= 2176

    def it_pebias(itx):
        return (not it_far(itx)) and (itx % PEB_MOD != 0)

    SB4 = [PS[0], PS[1], PS[2], PS[7]]
    srot = [0]
    it_bank = {}

    def sbank_next():
        return PS[7]

    def emit_S(itx):
        hd, s, j, first, last = iters[itx]
        ka_, qa_, wm_ = Kaug[hd % 2], Qaug[hd % 2], Wm[hd % 2]
        bank = PS[itx % 3]
        it_bank[itx] = bank
        delta0 = (2 * s + 1 - j) * 256
        peb = it_pebias(itx)

        def mm(h):
            r = None
            for kk in range(2):
                kt = 2 * j + (1 - kk)
                r = h.matmul(bank.ap()[:, kk * 256:(kk + 1) * 256], lhsT=ka_.ap()[0:80, kt * 128:(kt + 1) * 128],
                             rhs=qa_.ap()[0:80, s * 256:(s + 1) * 256], start=True, stop=not peb)
                if peb:
                    o_ = delta0 + 128 * kk
                    r = h.matmul(bank.ap()[:, kk * 256:(kk + 1) * 256], lhsT=ident.ap(), rhs=wm_.ap()[:, o_:o_ + 256], start=False, stop=True)
            return r
        S.add('pe', mm, [ka_, qa_] + ([wm_, ident] if peb else []), [bank])

    pend_norm = []

    def emit_rest(itx):
        hd, s, j, first, last = iters[itx]
        wm_ = Wm[hd % 2]
        bank = it_bank.pop(itx)
        sp_ = SpA[itx % 4]
        pt_ = PtA[itx % 4]
        own = 2 * s + 1
        delta0 = (own - j) * 256
        if it_far(itx):
            S.add('act', lambda h: h.activation(out=pt_.ap(), in_=bank.ap(), func=AF.Exp, bias=rb31.ap()[:, 8 + hd:9 + hd], scale=1.0),
                  [bank, rb31], [pt_])
        elif it_pebias(itx):
            S.add('act', lambda h: h.activation(out=pt_.ap(), in_=bank.ap(), func=AF.Exp), [bank], [pt_])
        else:
            w = wm_.ap()
            in1 = bass.AP(w.tensor, w.offset + delta0, [list(w.ap[0]), [128, 2], [1, 256]])
            S.add('dve', lambda h: h.tensor_tensor(out=sp_.ap().rearrange("p (k q) -> p k q", k=2),
                                                   in0=bank.ap().rearrange("p (k q) -> p k q", k=2), in1=in1, op=ALU.add),
                  [bank, wm_], [sp_])
            S.add('act', lambda h: h.activation(out=pt_.ap(), in_=sp_.ap(), func=AF.Exp), [sp_], [pt_])
        par_ = (hd * 8 + s) % 2
        ob = [PS[3], PS[4]] if par_ == 0 else [PS[5], PS[6]]
        dn_ = denA[2 + par_]

        def pv(h):
            r = None
            for kk in range(2):
                kt = 2 * j + (1 - kk)
                for q2 in range(2):
                    r = h.matmul(ob[q2].ap()[:, 0:65], lhsT=pt_.ap()[:, kk * 256 + q2 * 128:kk * 256 + q2 * 128 + 128],
                                 rhs=Vb.ap()[:, kt, hd, :], start=(first and kk == 0), stop=(last and kk == 1))
            return r
        S.add('pe', pv, [pt_, Vb], [ob[0], ob[1]])
        while pend_norm and pend_norm[0][0] <= itx:
            pend_norm.pop(0)[1]()
        if last:
            def norm():
                for q2 in range(2):
                    qt = 2 * s + q2
                    S.add('dve', lambda h, q2=q2: h.reciprocal(out=dn_.ap()[:, 8 + q2:9 + q2], in_=ob[q2].ap()[:, 64:65]), [ob[q2]], [dn_])
                    S.add('dve', lambda h, q2=q2, qt=qt: h.tensor_scalar(out=Ob.ap()[:, qt, hd * 64:(hd + 1) * 64], in0=ob[q2].ap()[:, 0:64],
                                                                         scalar1=dn_.ap()[:, 8 + q2:9 + q2], scalar2=None, op0=ALU.mult),
                          [ob[q2], dn_], [Ob.sl(qt)])
            n_next = 2 * (s + 1) + 2 if s < 7 else 2
            pend_norm.append((itx + min(4, n_next), norm))

    LOOK = 2
    n_it = len(iters)
    prep0 = moba_head_prep(0)
    swa_front(0)
    for it in range(len(swa_its)):
        if it == 12:
            prep0[3]()
        if it + 1 < len(swa_its):
            swa_front(it + 1)
        swa_back(it)

    if debug == 'p2a':
        dump(Oa.ap().rearrange("p t c -> p (t c)"), 8192)
        return finish()

    for i in range(2):
        dma('sp', f'c_blkoh{i}', Kaug[i].ap()[64:80, :], blkoh_d.ap(), [blkoh_d], [Kaug[i]])
    prep0[4]()
    prep0[1]()
    prep0[2]()
    events = {}

    def at(itx_, f):
        events.setdefault(itx_, []).append(f)

    bgb = [sb("bgb", [128, 2048], F32, o_) for o_ in (183744, 196288, 204480)]
    ev_i = 10
    for pass_ in (1, 2):
        ld_, ac_, fin_ = mod_pass(pass_ * 2048, 2048, bgb[0:2], bgb[2], f'mp{pass_}', lambda: PS[7], eng='pool', dq='pool')
        at(ev_i, lambda ld_=ld_: (ld_(0), ld_(1)))
        for k in range(9):
            def stp(k=k, ld_=ld_, ac_=ac_):
                ac_(k)
                if k + 2 < 9:
                    ld_(k + 2)
            at(ev_i + 12 * (k + 1), stp)
        at(ev_i + 12 * 10 + 60, fin_)
        ev_i += 12 * 10 + 70

    wgate = sb("wgate", [128, 8, 2048], BF16, 4096)
    wbr = sb("wbr", [128, 8, D], BF16, 70144)

    def prefetch_3a_weights():
        load_w(wbr, wba_d.ap().rearrange("(k p) n -> p k n", p=128), D, 'w_br', wba_d, kchunks=4, dst_k0=0)
        load_w(wbr, wbb_d.ap().rearrange("(k p) n -> p k n", p=128), D, 'w_br', wbb_d, kchunks=4, dst_k0=4)
        load_w(wgate, wg_d.ap().rearrange("(k p) n -> p k n", p=128), 2048, 'w_gate', wg_d)

    for itx in range(n_it + LOOK):
        if itx < n_it:
            hd, s_, j_ = iters[itx][0], iters[itx][1], iters[itx][2]
            if hd == 6 and s_ == 5 and j_ == 0:
                at(itx, prefetch_3a_weights)
            if s_ == 4 and j_ == 0 and hd + 1 < 8:
                st = moba_head_prep(hd + 1)
                for d_, f_ in zip((0, 16, 26), st):
                    at(itx + d_, f_)
            for f_ in events.pop(itx, []):
                f_()
            emit_S(itx)
        if itx - LOOK >= 0:
            emit_rest(itx - LOOK)
    while pend_norm:
        pend_norm.pop(0)[1]()
    assert not events, sorted(events)

    if debug in ('p2b', 'p2b_nolate'):
        dump(Ob.ap().rearrange("p t c -> p (t c)"), 8192)
        return finish()

    mergedT = sb("mergedT", [128, 8, 2048], BF16, 36864)
    OT = [sb("OT", [128, 8, 512], BF16, 86528 + i * 8192) for i in range(2)]
    sig = [sb("sig", [128, 512], F32, 102912 + i * 2048) for i in range(8)]
    assert 119296 <= O_W1
    O_F3 = O_W1 + 32768
    G1bc3 = sb("G1bc3", [128, D], F32, O_F3)
    SH1bc3 = sb("SH1bc3", [128, D], F32, O_F3 + 4096)
    xt3 = [sb("xt3", [128, D], F32, O_F3 + 8192 + i * 4096) for i in range(2)]
    hb3 = [sb("hb3", [128, D], BF16, O_F3 + 16384 + i * 2048) for i in range(2)]
    hT3 = [sb("hT3", [128, 8, 512], BF16, O_F3 + 20480 + i * 8192) for i in range(2)]
    g1bc3 = sb("g1bc3", [128, D], F32, O_F3 + 36864)
    load_mod_bc(SH1bc3, 0, 'c_sh1')
    load_mod_bc(G1bc3, 1, 'c_g1a')
    dma('sp', 'c_g1b', g1bc3.ap(), bcast_rows(g1_d.ap()), [g1_d], [g1bc3])
    S.add('dve', lambda h: h.scalar_tensor_tensor(out=G1bc3.ap(), in0=G1bc3.ap(), scalar=1.0, in1=g1bc3.ap(),
                                                  op0=ALU.add, op1=ALU.mult), [G1bc3, g1bc3], [G1bc3])
    wout = sb("wout", [128, 8, D], BF16, 193152)
    load_w(wout, wout_d.ap().rearrange("(k p) n -> p k n", p=128), D, 'w_out', wout_d)

    def p3_frontend(g):
        res = []
        for t in range(4):
            tile_i = g * 4 + t
            e_, t_ = frontend(x_own.ap()[tile_i * 128:(tile_i + 1) * 128, :], [x_own], xt3[tile_i % 2], hb3[tile_i % 2], hT3[g % 2], t,
                              G1bc3, SH1bc3, 0, xkey=f'xt3{tile_i % 2}')

            def tr2(t_=t_, tile_i=tile_i, t=t, g=g):
                t_()
                pvv = ps_bf(1).rearrange("p (c t) -> p c t", c=8)

                def tr(h):
                    r = None
                    for cc in range(4):
                        r = h.transpose(out=pvv[:, cc, :], in_=Oa.ap()[:, tile_i, cc * 128:(cc + 1) * 128], identity=ident.ap())
                    for cc in range(4):
                        r = h.transpose(out=pvv[:, 4 + cc, :], in_=Ob.ap()[:, tile_i, cc * 128:(cc + 1) * 128], identity=ident.ap())
                    return r
                S.add('pe', tr, [Oa.sl(tile_i), Ob.sl(tile_i), ident], [PS[1]])
                S.add('dve', lambda h: h.tensor_copy(out=OT[g % 2].ap()[:, :, t * 128:(t + 1) * 128], in_=pvv), [PS[1]], [OT[g % 2]])
            res.append((e_, tr2))
        return res

    p3_rr = [0]

    def p3_bank():
        b_ = PS[2 + (p3_rr[0] % 6)]
        p3_rr[0] += 1
        return b_

    def p3_units(g):
        hTb, OTb = hT3[g % 2], OT[g % 2]
        units = []
        for oc in range(8):
            def unit(oc=oc):
                sg = sig[4 * (oc % 2):4 * (oc % 2) + 4]
                b0, b1, b2, b3 = p3_bank(), p3_bank(), p3_bank(), p3_bank()

                def mm_a(h):
                    r = None
                    for k in range(4):
                        r = h.matmul(b0.ap(), lhsT=wbr.ap()[:, k, oc * 128:(oc + 1) * 128], rhs=OTb.ap()[:, k, :], start=(k == 0), stop=(k == 3))
                    return r

                def mm_b(h):
                    r = None
                    for k in range(4):
                        r = h.matmul(b1.ap(), lhsT=wbr.ap()[:, 4 + k, oc * 128:(oc + 1) * 128], rhs=OTb.ap()[:, 4 + k, :], start=(k == 0), stop=(k == 3))
                    return r

                def mm_ga(h):
                    r = None
                    for k in range(8):
                        r = h.matmul(b2.ap(), lhsT=wgate.ap()[:, k, oc * 128:(oc + 1) * 128], rhs=hTb.ap()[:, k, :], start=(k == 0), stop=(k == 7))
                    return r

                def mm_gb(h):
                    r = None
                    for k in range(8):
                        r = h.matmul(b3.ap(), lhsT=wgate.ap()[:, k, D + oc * 128:D + (oc + 1) * 128], rhs=hTb.ap()[:, k, :], start=(k == 0), stop=(k == 7))
                    return r
                S.add('pe', mm_ga, [wgate, hTb], [b2])
                S.add('act', lambda h: h.activation(out=sg[0].ap(), in_=b2.ap(), func=AF.Sigmoid), [b2], [sg[0]])
                S.add('pe', mm_gb, [wgate, hTb], [b3])
                S.add('act', lambda h: h.activation(out=sg[1].ap(), in_=b3.ap(), func=AF.Sigmoid), [b3], [sg[1]])
                S.add('pe', mm_a, [wbr, OTb], [b0])
                S.add('dve', lambda h: h.tensor_tensor(out=sg[2].ap(), in0=b0.ap(), in1=sg[0].ap(), op=ALU.mult), [b0, sg[0]], [sg[2]])
                S.add('pe', mm_b, [wbr, OTb], [b1])
                S.add('dve', lambda h: h.tensor_tensor(out=sg[3].ap(), in0=b1.ap(), in1=sg[1].ap(), op=ALU.mult), [b1, sg[1]], [sg[3]])
                S.add('pool', lambda h: h.tensor_tensor(out=mergedT.ap()[:, oc, g * 512:(g + 1) * 512], in0=sg[2].ap(), in1=sg[3].ap(),
                                                        op=ALU.add), [sg[2], sg[3]], [mergedT.sl(oc)])
            units.append(unit)
        return units

    for e_, t_ in p3_frontend(0):
        e_()
        t_()
    x1 = sb("x1", [128, 16, D], F32, O_W1)
    X1_EARLY = (8, 9, 10, 11, 12, 13, 14)

    def early_x1():
        for t in X1_EARLY:
            dma('sp', f'x1ld{t % 4}', x1.ap()[:, t, :], x_own.ap()[t * 128:(t + 1) * 128, :], [x_own], [x1.sl(t)])

    for g in range(4):
        units = p3_units(g)
        extras = []
        if g == 3:
            extras.append((1, early_x1))
        if g + 1 < 4:
            for t, (e_, t_) in enumerate(p3_frontend(g + 1)):
                extras.append((2 * t, e_))
                extras.append((2 * t + 1, t_))
        interleave(units, extras)

    h2T = sb("h2T", [128, 8, 2048], BF16, 4096)
    WB = (69632, 86016, 36864, 53248)
    wmi = [sb("wmi", [128, 8, 512], BF16, o_) for o_ in WB]
    wmo = [sb("wmo", [128, 4, D], BF16, o_ + 8192) for o_ in WB]
    aT = [sb("aT", [128, 4, 512], BF16, 102400 + i * 4096) for i in range(2)]
    O_G = O_W1 + 65536
    gbc = sb("gbc", [128, D], F32, O_G)
    tmpz = [sb("tmpz", [128, 512], F32, O_G + 4096 + i * 2048) for i in range(2)]
    rbuf = [sb("rbuf", [128, 512], F32, O_G + 8192 + i * 2048) for i in range(2)]
    O_4 = 102400
    O_6 = 197248
    G2bc = sb("G2bc", [128, D], F32, O_4)
    SH2bc = sb("SH2bc", [128, D], F32, O_4 + 4096)
    t4 = sb("t4", [128, D], F32, O_4 + 8192)
    hb4s = [sb("hb4", [128, D], BF16, O_4 + 12288), sb("hb4", [128, D], BF16, O_G + 4096)]
    wmi_v = wmi_d.ap().rearrange("(k p) n -> p k n", p=128)
    wmo_v = wmo_d.ap().rearrange("(k p) n -> p k n", p=128)

    def load_piece(p):
        load_w(wmi[p % 4], wmi_v[:, :, p * 512:(p + 1) * 512], 512, f'w_mi{p % 4}', wmi_d)
        load_w(wmo[p % 4], wmo_v[:, p * 4:(p + 1) * 4, :], D, f'w_mo{p % 4}', wmo_d, kchunks=4)

    load_mod_bc(gbc, 2, 'c_gate')
    for k in range(8):
        S.add('pool', lambda h, k=k: h.tensor_tensor(out=wout.ap()[:, k, :], in0=wout.ap()[:, k, :], in1=gbc.ap(), op=ALU.mult),
              [wout.sl(k), gbc], [wout.sl(k)])
    load_mod_bc(SH2bc, 3, 'c_sh2')
    load_mod_bc(G2bc, 4, 'c_g2a')
    dma('sp', 'c_g2b', t4.ap(), bcast_rows(g2_d.ap()), [g2_d], [t4])
    S.add('dve', lambda h: h.scalar_tensor_tensor(out=G2bc.ap(), in0=G2bc.ap(), scalar=1.0, in1=t4.ap(),
                                                  op0=ALU.add, op1=ALU.mult), [G2bc, t4], [G2bc])
    for t in range(16):
        if t not in X1_EARLY:
            dma('sp', f'x1ld{t % 4}', x1.ap()[:, t, :], x_own.ap()[t * 128:(t + 1) * 128, :], [x_own], [x1.sl(t)])

    def p4_elem(t):
        xr = x1.sl(t)
        x_ap = x1.ap()[:, t, :]
        st_ = stat2[t % 2]
        hb4 = hb4s[t % 2]
        S.add('act', lambda h: h.activation(out=hb4.ap(), in_=x_ap, func=AF.Square, accum_out=st_.ap()[:, 0:1]), [xr], [hb4, st_])
        S.add('act', lambda h: h.activation(out=st_.ap()[:, 1:2], in_=st_.ap()[:, 0:1], func=AF.Sqrt,
                                            bias=epsb.ap()[:, 0:1], scale=1.0 / D), [st_, epsb], [st_])
        S.add('dve', lambda h: h.reciprocal(out=st_.ap()[:, 2:3], in_=st_.ap()[:, 1:2]), [st_], [st_])
        S.add('dve', lambda h: h.scalar_tensor_tensor(out=t4.ap(), in0=x_ap, scalar=st_.ap()[:, 2:3], in1=G2bc.ap(),
                                                      op0=ALU.mult, op1=ALU.mult), [xr, st_, G2bc], [t4])
        S.add('pool', lambda h: h.tensor_tensor(out=hb4.ap(), in0=t4.ap(), in1=SH2bc.ap(), op=ALU.add), [t4, SH2bc], [hb4])

    def p4_tr(t):
        hb4 = hb4s[t % 2]
        pvv = ps_bf(4).rearrange("p (c t) -> p c t", c=8)

        def tr(h):
            r = None
            for c in range(8):
                r = h.transpose(out=pvv[:, c, :], in_=hb4.ap()[:, c * 128:(c + 1) * 128], identity=ident.ap())
            return r
        S.add('pe', tr, [hb4, ident], [PS[4]])
        S.add('act', lambda h: h.copy(out=h2T.ap()[:, :, t * 128:(t + 1) * 128], in_=pvv), [PS[4]], [h2T])

    load_piece(0)
    zc = 0
    for t in range(16):
        for nh in range(2):
            b = PS[zc % 4]
            zc += 1

            def mm(h, t=t, nh=nh, b=b):
                r = None
                for k in range(8):
                    r = h.matmul(b.ap(), lhsT=mergedT.ap()[:, k, t * 128:(t + 1) * 128], rhs=wout.ap()[:, k, nh * 512:(nh + 1) * 512],
                                 start=(k == 0), stop=(k == 7))
                return r
            S.add('pe', mm, [mergedT, wout], [b])
            S.add('dve', lambda h, t=t, nh=nh, b=b: h.tensor_tensor(out=x1.ap()[:, t, nh * 512:(nh + 1) * 512], in0=b.ap(),
                                                                    in1=x1.ap()[:, t, nh * 512:(nh + 1) * 512], op=ALU.add),
                  [b, x1.sl(t)], [x1.sl(t)])
        if t >= 1:
            p4_elem(t - 1)
        if t >= 2:
            p4_tr(t - 2)
    p4_elem(15)
    p4_tr(14)
    p4_tr(15)

    if debug == 'p3':
        dump(x1.ap()[:, 0:8, :].rearrange("p t c -> p (t c)"), 8192)
        return finish()

    gbc2 = sb("gbc2", [128, D], F32, O_G)
    load_mod_bc(gbc2, 5, 'c_gate')
    gfbc = sb("gfbc", [128, D], F32, O_6)
    ot = [sb("ot", [128, D], F32, O_6 + 4096 + i * 4096) for i in range(2)]
    junk6 = sb("junk6", [128, D], BF16, O_6 + 12288)

    def p6_tile(t):
        xr = x1.sl(t)
        x_ap = x1.ap()[:, t, :]
        o_ = ot[t % 2]
        st_ = stat2[t % 2]
        S.add('act', lambda h: h.activation(out=junk6.ap(), in_=x_ap, func=AF.Square, accum_out=st_.ap()[:, 0:1]), [xr], [junk6, st_])
        S.add('act', lambda h: h.activation(out=st_.ap()[:, 1:2], in_=st_.ap()[:, 0:1], func=AF.Sqrt,
                                            bias=epsb.ap()[:, 0:1], scale=1.0 / D), [st_, epsb], [st_])
        S.add('dve', lambda h: h.reciprocal(out=st_.ap()[:, 2:3], in_=st_.ap()[:, 1:2]), [st_], [st_])
        S.add('dve', lambda h: h.scalar_tensor_tensor(out=o_.ap(), in0=x_ap, scalar=st_.ap()[:, 2:3], in1=gfbc.ap(),
                                                      op0=ALU.mult, op1=ALU.mult), [xr, st_, gfbc], [o_])
        dma('sp', f'ost{t % 2}', out_d.ap()[t * 128:(t + 1) * 128, :], o_.ap(), [o_], [out_d.el(t * 128 * D, (t + 1) * 128 * D)])

    yc = 0
    uc = 0

    def fold_gate2(p):
        wo = wmo[p % 4]
        for k in range(4):
            S.add('pool', lambda h, k=k: h.tensor_tensor(out=wo.ap()[:, k, :], in0=wo.ap()[:, k, :], in1=gbc2.ap(), op=ALU.mult),
                  [wo.sl(k), gbc2], [wo.sl(k)])

    fold_gate2(0)
    load_piece(1)
    fold_gate2(1)
    load_piece(2)
    fold_gate2(2)
    NPC = 8
    pg = [(p, g) for p in range(NPC) for g in range(4)]

    def emit_U(i):
        p, g = pg[i]
        nonlocal_uc = uc_box
        wi = wmi[p % 4]
        aTb = aT[i % 2]
        for hc in range(4):
            b = PS[4 + (nonlocal_uc[0] % 4)]
            rb_ = rbuf[nonlocal_uc[0] % 2]
            nonlocal_uc[0] += 1

            def mm(h, hc=hc, b=b):
                r = None
                for k in range(8):
                    r = h.matmul(b.ap(), lhsT=wi.ap()[:, k, hc * 128:(hc + 1) * 128], rhs=h2T.ap()[:, k, g * 512:(g + 1) * 512],
                                 start=(k == 0), stop=(k == 7))
                return r
            S.add('pe', mm, [wi, h2T], [b])
            S.add('act', lambda h, b=b, rb_=rb_: h.activation(out=rb_.ap(), in_=b.ap(), func=AF.Relu), [b], [rb_])
            S.add('act', lambda h, hc=hc, rb_=rb_: h.activation(out=aTb.ap()[:, hc, :], in_=rb_.ap(), func=AF.Square),
                  [rb_], [aTb.sl(hc)])

    def emit_Y(i):
        p, g = pg[i]
        wo = wmo[p % 4]
        aTb = aT[i % 2]
        if g == 0:
            if p + 3 < NPC:
                load_piece(p + 3)
                fold_gate2(p + 3)
            if p == NPC - 1:
                dma('sp', 'c_gf', gfbc.ap(), bcast_rows(gf_d.ap()), [gf_d], [gfbc])
        for t in range(4):
            tt = g * 4 + t
            for nh in range(2):
                b = PS[yc_box[0] % 4]
                yc_box[0] += 1

                def mm(h, t=t, nh=nh, b=b):
                    r = None
                    for k in range(4):
                        r = h.matmul(b.ap(), lhsT=aTb.ap()[:, k, t * 128:(t + 1) * 128], rhs=wo.ap()[:, k, nh * 512:(nh + 1) * 512],
                                     start=(k == 0), stop=(k == 3))
                    return r
                S.add('pe', mm, [aTb, wo], [b])
                S.add('dve', lambda h, tt=tt, nh=nh, b=b: h.tensor_tensor(out=x1.ap()[:, tt, nh * 512:(nh + 1) * 512], in0=b.ap(),
                                                                          in1=x1.ap()[:, tt, nh * 512:(nh + 1) * 512], op=ALU.add),
                      [b, x1.sl(tt)], [x1.sl(tt)])
            if p == NPC - 1:
                p6_tile(tt)

    uc_box, yc_box = [0], [0]
    emit_U(0)
    for i in range(len(pg)):
        if i + 1 < len(pg):
            emit_U(i + 1)
        emit_Y(i)
    return finish()


def _consts():
    ident = np.eye(128, dtype=np.float32).astype(ml_dtypes.bfloat16)
    ohm = np.zeros((33, LT_M), np.float32)
    d = np.arange(LT_M) - 255
    bk = t5_bucket_np(d)
    for i in range(LT_M):
        if d[i] < 0:
            ohm[32, i] = 1.0
        else:
            ohm[bk[i], i] = 1.0
    oha = np.zeros((33, LT_A), np.float32)
    d = np.arange(LT_A) - 255
    bk = t5_bucket_np(d)
    for i in range(LT_A):
        if 0 <= d[i] < 128:
            oha[bk[i], i] = 1.0
        else:
            oha[32, i] = 1.0
    blkoh = np.zeros((16, 4096), np.float32)
    for j in range(16):
        blkoh[j, j * 256:(j + 1) * 256] = 1.0
    return ident, ohm.astype(ml_dtypes.bfloat16), oha.astype(ml_dtypes.bfloat16), blkoh.astype(ml_dtypes.bfloat16)


def make_in_maps(x, c, ada_w, ada_b, norm1_g, norm2_g, w_in, attn_sinks, rel_bias, w_branch_a, w_branch_b, w_out,
                 w_mlp_in, w_mlp_out, final_g):
    f = lambda a: np.ascontiguousarray(np.asarray(a, dtype=np.float32))
    x, c = f(x), f(c)
    w_in0 = f(w_in)[0]
    qa = w_in0[:, 0:512]
    qa_perm = np.concatenate([np.concatenate([qa[:, j * 64:(j + 1) * 64], qa[:, (4 + j) * 64:(5 + j) * 64]], axis=1) for j in range(4)], axis=1)
    wqkv = np.concatenate([qa_perm, w_in0[:, 768:1280], w_in0[:, 1280:1792], w_in0[:, 512:640], w_in0[:, 1792:2304], w_in0[:, 640:768]], axis=1)
    wg = w_in0[:, 2304:4352]
    ident, ohm, oha, blkoh = _consts()
    shared = dict(
        ada_w=f(ada_w)[0], ada_b=f(ada_b)[0:1], g1=f(norm1_g)[0:1], g2=f(norm2_g)[0:1], gf=f(final_g).reshape(1, D),
        wqkv=np.ascontiguousarray(wqkv), wg=np.ascontiguousarray(wg), sinks=f(attn_sinks)[0:1], relb=f(rel_bias),
        wba=f(w_branch_a)[0], wbb=f(w_branch_b)[0], wout=f(w_out)[0], wmi=f(w_mlp_in)[0], wmo=f(w_mlp_out)[0],
        ident=ident, ohm=ohm, oha=oha, blkoh=blkoh)
    in_maps = []
    for core in range(8):
        b, par = core // 2, core % 2
        own_blocks = [2 * i + par for i in range(8)]
        xo = np.concatenate([x[b, blk * 256:(blk + 1) * 256] for blk in own_blocks], axis=0)
        oth = []
        for v in range(0, 16, 2):
            a = v - 1 if par == 0 else v
            oth.append(np.zeros((256, D), np.float32) if a < 0 else x[b, a * 256:(a + 1) * 256])
        xoth = np.concatenate(oth, axis=0)
        m = dict(shared)
        m.update(x_own=np.ascontiguousarray(xo), x_oth=np.ascontiguousarray(xoth),
                 ccol=np.ascontiguousarray(c[b].reshape(8, 128).T), flag=np.full((128, 1), 1.0 if par == 0 else 0.0, np.float32))
        in_maps.append(m)
    return in_maps


_PROG = {}


def kernel(**inputs):
    if 'nc' not in _PROG:
        _PROG['nc'], _PROG['info'] = build_program(None)
    nc = _PROG['nc']
    in_maps = make_in_maps(**inputs)
    res = run_bass_kernel_spmd(nc, in_maps, core_ids=list(range(8)))
    out = np.zeros((4, 4096, D), np.float32)
    for core in range(8):
        b, par = core // 2, core % 2
        o = res.results[core]["out"]
        for i in range(8):
            blk = 2 * i + par
            out[b, blk * 256:(blk + 1) * 256] = o[i * 256:(i + 1) * 256]
    return out
```
